# Optimizing a Trainium2 kernel written in Bass

```python
import math
import jax, jax.numpy as jnp
from jax import lax
import numpy as np

D_MODEL = 4096
BATCH = 1
SEQ = 16384
DEPTH = 4

GRID_W = 64
CTX_LEN = 256
N_MIXERS = 2
NORM_EPS = 1e-6
S5_GROUP = 16
S5_GROUPS = D_MODEL // S5_GROUP
S5_STATE = 64
S5_DIRS = 2
SCAN_CHUNK = 128
DT_MIN = 1e-3
DT_MAX = 1e-1
NA_HEADS = 32
NA_HEAD_DIM = D_MODEL // NA_HEADS
NA_KH = 8
NA_KW = 16
FFN_DIM = 4096
N_EXPERTS = 8
TOP_K = 2
EXPERT_DIM = 1024
N_EVEN_LAYERS = (DEPTH + 1) // 2
N_ODD_LAYERS = DEPTH // 2

kernel_name = "hybrid_s5_natten_moe_dit"


def _rms_norm(x, g):
    xf = x.astype(jnp.float32)
    y = xf * lax.rsqrt(jnp.mean(xf * xf, axis=-1, keepdims=True) + NORM_EPS)
    return (y * g.astype(jnp.float32)).astype(x.dtype)


def _adaln(cond, w, b):
    m = (jax.nn.silu(cond) @ w + b)[:, None, :]
    return jnp.split(m, 6, axis=-1)


def _modulate(h, shift, scale):
    return h * (1 + scale) + shift


def _swiglu(h, w_gu, w_down):
    gate, up = jnp.split(h @ w_gu, 2, axis=-1)
    return (jax.nn.silu(gate) * up) @ w_down


def _moe(h, w_router, w_gu, w_down):
    logits = (h @ w_router).astype(jnp.float32)
    top_val, top_idx = lax.top_k(logits, TOP_K)
    top_w = jax.nn.softmax(top_val, axis=-1)
    gates = jnp.einsum('btk,btke->bte', top_w,
                       jax.nn.one_hot(top_idx, N_EXPERTS, dtype=jnp.float32)).astype(h.dtype)
    out = jnp.zeros_like(h)
    for e in range(N_EXPERTS):
        out = out + gates[..., e:e + 1] * _swiglu(h, w_gu[e], w_down[e])
    return out


def _ssm_combine(left, right):
    a_l, b_l = left
    a_r, b_r = right
    return a_l * a_r, a_r * b_l + b_r


def _s5_scan(u, h0, lam, dt, b_mat, c_mat):
    bsz, length = u.shape[:2]
    n_chunks = length // SCAN_CHUNK
    lam_dt = lam * dt[:, None]
    a_bar = jnp.exp(lam_dt)
    b_bar = ((a_bar - 1.0) / lam)[..., None] * b_mat
    steps = jnp.arange(1, SCAN_CHUNK + 1, dtype=jnp.float32)[:, None, None]
    a_pow = jnp.exp(lam_dt[None] * steps)
    u_chunks = jnp.moveaxis(u.reshape(bsz, n_chunks, SCAN_CHUNK, S5_GROUPS, S5_GROUP), 1, 0)

    def chunk_step(h, u_c):
        bu = jnp.einsum('btgc,gnc->btgn', u_c.astype(jnp.complex64), b_bar)
        a = jnp.broadcast_to(a_bar, bu.shape)
        _, h_loc = lax.associative_scan(_ssm_combine, (a, bu), axis=1)
        h_all = h_loc + a_pow[None] * h[:, None]
        y = jnp.einsum('btgn,gcn->btgc', h_all, c_mat).real
        return h_all[:, -1], y

    h_last, y = lax.scan(chunk_step, h0, u_chunks)
    return jnp.moveaxis(y, 0, 1).reshape(bsz, length, S5_GROUPS, S5_GROUP), h_last


def _s5_glu(y, w_glu):
    g = jax.nn.gelu(y)
    a, b = jnp.split(g @ w_glu, 2, axis=-1)
    return a * jax.nn.sigmoid(b)


def _s5_mixer(hx, hc, a_re, a_im, log_dt, b_re, b_im, c_re, c_im, d_skip, w_glu, with_ctx_out):
    bsz, seq, _ = hx.shape
    f32 = jnp.float32
    ux = hx.astype(f32).reshape(bsz, seq, S5_GROUPS, S5_GROUP)
    uc = hc.astype(f32).reshape(bsz, CTX_LEN, S5_GROUPS, S5_GROUP)
    d_g = d_skip.astype(f32).reshape(S5_GROUPS, S5_GROUP)
    yx = d_g * ux
    yc = d_g * uc
    for direction in range(S5_DIRS):
        lam = lax.complex(a_re[direction].astype(f32), a_im[direction].astype(f32))
        dt = jnp.exp(log_dt[direction].astype(f32))
        b_mat = lax.complex(b_re[direction].astype(f32), b_im[direction].astype(f32))
        c_mat = lax.complex(c_re[direction].astype(f32), c_im[direction].astype(f32))
        rev = direction == 1
        uc_d = uc[:, ::-1] if rev else uc
        ux_d = ux[:, ::-1] if rev else ux
        h0 = jnp.zeros((bsz, S5_GROUPS, S5_STATE), jnp.complex64)
        yc_d, h_ctx = _s5_scan(uc_d, h0, lam, dt, b_mat, c_mat)
        yx_d, _ = _s5_scan(ux_d, h_ctx, lam, dt, b_mat, c_mat)
        if rev:
            yc_d = yc_d[:, ::-1]
            yx_d = yx_d[:, ::-1]
        yx = yx + yx_d
        yc = yc + yc_d
    out_x = _s5_glu(yx.reshape(bsz, seq, D_MODEL).astype(hx.dtype), w_glu)
    out_c = _s5_glu(yc.reshape(bsz, CTX_LEN, D_MODEL).astype(hc.dtype), w_glu) if with_ctx_out else None
    return out_x, out_c


def _na_mixer(hx, hc, w_qkv, w_o, rpb, with_ctx_out):
    bsz, seq, _ = hx.shape
    rows = seq // GRID_W
    kh = min(NA_KH, rows)
    scale = NA_HEAD_DIM ** -0.5
    qkv = (hx @ w_qkv).reshape(bsz, seq, 3, NA_HEADS, NA_HEAD_DIM)
    qg = qkv[:, :, 0].reshape(bsz, rows, GRID_W, NA_HEADS, NA_HEAD_DIM)
    kg = qkv[:, :, 1].reshape(bsz, rows, GRID_W, NA_HEADS, NA_HEAD_DIM)
    vg = qkv[:, :, 2].reshape(bsz, rows, GRID_W, NA_HEADS, NA_HEAD_DIM)
    qkv_c = (hc @ w_qkv).reshape(bsz, CTX_LEN, 3, NA_HEADS, NA_HEAD_DIM)
    qc, kc, vc = qkv_c[:, :, 0], qkv_c[:, :, 1], qkv_c[:, :, 2]

    cols = jnp.arange(GRID_W)
    col_start = jnp.clip(cols - NA_KW // 2, 0, GRID_W - NA_KW)
    col_idx = col_start[:, None] + jnp.arange(NA_KW)[None, :]
    col_rel = col_idx - cols[:, None] + (NA_KW - 1)
    col_bias = rpb[:, :, col_rel]
    n_win = kh * NA_KW

    def row_block(r):
        r0 = jnp.clip(r - kh // 2, 0, rows - kh)
        k_band = lax.dynamic_slice_in_dim(kg, r0, kh, axis=1)
        v_band = lax.dynamic_slice_in_dim(vg, r0, kh, axis=1)
        k_win = k_band[:, :, col_idx]
        v_win = v_band[:, :, col_idx]
        q_r = lax.dynamic_index_in_dim(qg, r, axis=1, keepdims=False)
        row_rel = r0 + jnp.arange(kh) - r + (NA_KH - 1)
        bias = jnp.take(col_bias, row_rel, axis=1).transpose(0, 2, 1, 3)
        s_win = jnp.einsum('bqhd,bjqkhd->bhqjk', q_r, k_win).astype(jnp.float32) * scale
        s_win = s_win + bias[None].astype(jnp.float32)
        s_ctx = jnp.einsum('bqhd,bchd->bhqc', q_r, kc).astype(jnp.float32) * scale
        s = jnp.concatenate([s_win.reshape(bsz, NA_HEADS, GRID_W, n_win), s_ctx], axis=-1)
        p = jax.nn.softmax(s, axis=-1).astype(vg.dtype)
        p_win = p[..., :n_win].reshape(bsz, NA_HEADS, GRID_W, kh, NA_KW)
        return (jnp.einsum('bhqjk,bjqkhd->bqhd', p_win, v_win)
                + jnp.einsum('bhqc,bchd->bqhd', p[..., n_win:], vc))

    o = lax.map(row_block, jnp.arange(rows))
    out_x = jnp.moveaxis(o, 0, 1).reshape(bsz, seq, D_MODEL) @ w_o
    out_c = None
    if with_ctx_out:
        s = jnp.einsum('bqhd,bkhd->bhqk', qc, kc).astype(jnp.float32) * scale
        p = jax.nn.softmax(s, axis=-1).astype(vc.dtype)
        out_c = jnp.einsum('bhqk,bkhd->bqhd', p, vc).reshape(bsz, CTX_LEN, D_MODEL) @ w_o
    return out_x, out_c


def setup_inputs(seed: int = 0) -> dict:
    key = jax.random.key(seed)
    ks = jax.random.split(key, 32)
    f32 = jnp.float32

    def nrm(k, shape, scale):
        return jax.random.normal(k, shape, f32) * scale

    ne, no = N_EVEN_LAYERS, N_ODD_LAYERS
    s5_shape = (ne, S5_DIRS, S5_GROUPS, S5_STATE)
    state_idx = jnp.arange(S5_STATE, dtype=f32)
    return {
        "x": nrm(ks[0], (BATCH, SEQ, D_MODEL), 1.0),
        "c": nrm(ks[1], (BATCH, D_MODEL), 1.0),
        "ctx": nrm(ks[2], (BATCH, CTX_LEN, D_MODEL), 1.0),
        "c_ctx": nrm(ks[3], (D_MODEL,), 1.0),
        "ada_w": nrm(ks[4], (DEPTH, D_MODEL, 6 * D_MODEL), 0.5 * D_MODEL ** -0.5),
        "ada_b": nrm(ks[5], (DEPTH, 6 * D_MODEL), 0.02),
        "norm_g": 1.0 + nrm(ks[6], (DEPTH, 4, D_MODEL), 0.05),
        "s5_a_re": -0.5 + nrm(ks[7], s5_shape, 0.01),
        "s5_a_im": math.pi * state_idx + nrm(ks[8], s5_shape, 0.01),
        "s5_log_dt": jax.random.uniform(ks[9], (ne, S5_DIRS, S5_GROUPS), f32,
                                        math.log(DT_MIN), math.log(DT_MAX)),
        "s5_b_re": nrm(ks[10], (ne, S5_DIRS, S5_GROUPS, S5_STATE, S5_GROUP), (2 * S5_GROUP) ** -0.5),
        "s5_b_im": nrm(ks[11], (ne, S5_DIRS, S5_GROUPS, S5_STATE, S5_GROUP), (2 * S5_GROUP) ** -0.5),
        "s5_c_re": nrm(ks[12], (ne, S5_DIRS, S5_GROUPS, S5_GROUP, S5_STATE), (2 * S5_STATE) ** -0.5),
        "s5_c_im": nrm(ks[13], (ne, S5_DIRS, S5_GROUPS, S5_GROUP, S5_STATE), (2 * S5_STATE) ** -0.5),
        "s5_d": nrm(ks[14], (ne, D_MODEL), 1.0),
        "s5_w_glu": nrm(ks[15], (ne, D_MODEL, 2 * D_MODEL), D_MODEL ** -0.5),
        "na_w_qkv": nrm(ks[16], (no, D_MODEL, 3 * D_MODEL), D_MODEL ** -0.5),
        "na_w_o": nrm(ks[17], (no, D_MODEL, D_MODEL), D_MODEL ** -0.5),
        "na_rpb": nrm(ks[18], (no, NA_HEADS, 2 * NA_KH - 1, 2 * NA_KW - 1), 0.02),
        "ffn_w_gu": nrm(ks[19], (ne, D_MODEL, 2 * FFN_DIM), D_MODEL ** -0.5),
        "ffn_w_down": nrm(ks[20], (ne, FFN_DIM, D_MODEL), FFN_DIM ** -0.5),
        "moe_w_router": nrm(ks[21], (no, D_MODEL, N_EXPERTS), D_MODEL ** -0.5),
        "moe_w_gu": nrm(ks[22], (no, N_EXPERTS, D_MODEL, 2 * EXPERT_DIM), D_MODEL ** -0.5),
        "moe_w_down": nrm(ks[23], (no, N_EXPERTS, EXPERT_DIM, D_MODEL), EXPERT_DIM ** -0.5),
    }


def reference(x, c, ctx, c_ctx, ada_w, ada_b, norm_g,
              s5_a_re, s5_a_im, s5_log_dt, s5_b_re, s5_b_im, s5_c_re, s5_c_im, s5_d, s5_w_glu,
              na_w_qkv, na_w_o, na_rpb,
              ffn_w_gu, ffn_w_down,
              moe_w_router, moe_w_gu, moe_w_down):
    for i in range(DEPTH):
        j = i // N_MIXERS
        with_ctx_out = i < DEPTH - 1
        sh_m, sc_m, g_m, sh_f, sc_f, g_f = _adaln(c, ada_w[i], ada_b[i])
        csh_m, csc_m, cg_m, csh_f, csc_f, cg_f = _adaln(c_ctx[None], ada_w[i], ada_b[i])

        hx = _modulate(_rms_norm(x, norm_g[i, 0]), sh_m, sc_m)
        hc = _modulate(_rms_norm(ctx, norm_g[i, 0]), csh_m, csc_m)
        if i % N_MIXERS == 0:
            yx, yc = _s5_mixer(hx, hc, s5_a_re[j], s5_a_im[j], s5_log_dt[j], s5_b_re[j], s5_b_im[j],
                               s5_c_re[j], s5_c_im[j], s5_d[j], s5_w_glu[j], with_ctx_out)
        else:
            yx, yc = _na_mixer(hx, hc, na_w_qkv[j], na_w_o[j], na_rpb[j], with_ctx_out)
        x = x + g_m * _rms_norm(yx, norm_g[i, 1])
        if with_ctx_out:
            ctx = ctx + cg_m * _rms_norm(yc, norm_g[i, 1])

        hx = _modulate(_rms_norm(x, norm_g[i, 2]), sh_f, sc_f)
        if with_ctx_out:
            hc = _modulate(_rms_norm(ctx, norm_g[i, 2]), csh_f, csc_f)
            h = jnp.concatenate([hc, hx], axis=1)
        else:
            h = hx
        if i % 2 == 0:
            y = _swiglu(h, ffn_w_gu[j], ffn_w_down[j])
        else:
            y = _moe(h, moe_w_router[j], moe_w_gu[j], moe_w_down[j])
        y = _rms_norm(y, norm_g[i, 3])
        if with_ctx_out:
            ctx = ctx + cg_f * y[:, :CTX_LEN]
            x = x + g_f * y[:, CTX_LEN:]
        else:
            x = x + g_f * y
    return x
```

```python
import math
import numpy as np
import ml_dtypes
import concourse.bass as bass
import concourse.mybir as mybir
from concourse.bass_utils import run_bass_kernel_spmd


F32 = mybir.dt.float32
BF16 = mybir.dt.bfloat16
I32 = mybir.dt.int32
AF = mybir.ActivationFunctionType
ALU = mybir.AluOpType

ENGS = ("pe", "act", "dve", "pool", "sp")


class Dep:
    __slots__ = ("name", "last_write", "reads")

    def __init__(self, name):
        self.name = name
        self.last_write = None
        self.reads = []


class Op:
    __slots__ = ("eng", "fn", "deps", "is_dma", "n_inst", "needed", "sem", "val", "idx")

    def __init__(self, eng, fn, is_dma, n_inst):
        self.eng = eng
        self.fn = fn
        self.deps = {}
        self.is_dma = is_dma
        self.n_inst = n_inst
        self.needed = False
        self.sem = None
        self.val = 0


class Sched:
    def __init__(self, nc):
        self.nc = nc
        self.ops = []
        self.engobj = {"pe": nc.tensor, "act": nc.scalar, "dve": nc.vector,
                       "pool": nc.gpsimd, "sp": nc.sync}

    def op(self, eng, fn, reads=(), writes=(), is_dma=False, n_inst=1):
        o = Op(eng, fn, is_dma, n_inst)
        idx = len(self.ops)
        o.idx = idx
        for d in reads:
            if d.last_write is not None:
                o.deps[d.last_write] = True
        for d in writes:
            if d.last_write is not None:
                o.deps.setdefault(d.last_write, False)
            for r in d.reads:
                if r != idx:
                    o.deps.setdefault(r, False)
        for d in reads:
            d.reads.append(idx)
        for d in writes:
            d.last_write = idx
            d.reads = []
        self.ops.append(o)
        return o

    def emit(self):
        nc = self.nc
        ops = self.ops
        for o in ops:
            keep = set()
            for di, raw in o.deps.items():
                p = ops[di]
                if (not p.is_dma) and (not o.is_dma) and p.eng == o.eng:
                    if p.eng == "pe" or not raw:
                        continue
                keep.add(di)
            o.deps = keep
            for di in keep:
                ops[di].needed = True
            if o.is_dma:
                o.needed = True
        sems = {}
        for e in ENGS:
            sems[e] = nc.alloc_semaphore("s_" + e)
        cnt = {e: 0 for e in ENGS}
        NDS = 24
        dsem = {q: [nc.alloc_semaphore("d_%s_%d" % (q, i)) for i in range(NDS)] for q in ("sp", "act", "pool")}
        dcnt = {q: [0] * NDS for q in dsem}
        drr = {q: 0 for q in dsem}
        waited = {e: {} for e in ENGS}
        for o in ops:
            eo = self.engobj[o.eng]
            need = {}
            for di in o.deps:
                p = ops[di]
                key = id(p.sem)
                if key not in need or need[key][1] < p.val:
                    need[key] = (p.sem, p.val)
            for key, (sem, val) in need.items():
                if waited[o.eng].get(key, 0) >= val:
                    continue
                eo.wait_ge(sem, val)
                waited[o.eng][key] = val
            slot = None
            if o.is_dma and o.needed:
                q = o.eng
                slot = drr[q]
                drr[q] = (slot + 1) % NDS
                if dcnt[q][slot] > 0 and waited[q].get(id(dsem[q][slot]), 0) < dcnt[q][slot]:
                    eo.wait_ge(dsem[q][slot], dcnt[q][slot])
                    waited[q][id(dsem[q][slot])] = dcnt[q][slot]
            insts = o.fn(eo)
            if not isinstance(insts, (list, tuple)):
                insts = [insts]
            assert len(insts) == o.n_inst, (len(insts), o.n_inst)
            if o.is_dma:
                if o.needed:
                    q = o.eng
                    for ins in insts:
                        ins.then_inc(dsem[q][slot], 16)
                    dcnt[q][slot] += 16 * len(insts)
                    o.sem = dsem[q][slot]
                    o.val = dcnt[q][slot]
            else:
                if o.needed:
                    insts[-1].then_inc(sems[o.eng], 1)
                    cnt[o.eng] += 1
                    o.sem = sems[o.eng]
                    o.val = cnt[o.eng]
        self.final = (sems, dsem)


NP_BF16 = ml_dtypes.bfloat16
EPS = 1e-6


class TT:
    def __init__(self, t, name):
        self.t = t
        self.name = name
        self.d = Dep(name)
        self._sub = {}

    def __getitem__(self, k):
        return self.t[k]

    def dk(self, k):
        if k not in self._sub:
            self._sub[k] = Dep("%s.%s" % (self.name, k))
        return self._sub[k]


class PB:
    def __init__(self):
        self.nc = bass.Bass("TRN2", target_bir_lowering=False)
        self.S = Sched(self.nc)
        self.outs = []
        self._n = 0

    def sb(self, name, shape, dt):
        t = TT(self.nc.alloc_sbuf_tensor(name, list(shape), dt), name)
        self._n += 1
        eng = "pool" if self._n % 2 else "dve"
        self.S.op(eng, lambda e: e.memset(t.t[:], 0), writes=[t.d])
        return t

    def ps(self, name, shape=(128, 512), dt=F32):
        t = TT(self.nc.alloc_psum_tensor(name, list(shape), dt), name)
        self.S.op("dve", lambda e: e.memset(t.t[:], 0.0), writes=[t.d])
        return t

    def din(self, name, shape, dt):
        return TT(self.nc.dram_tensor(name, list(shape), dt, kind="ExternalInput").ap(), name)

    def dout(self, name, shape, dt):
        t = TT(self.nc.dram_tensor(name, list(shape), dt, kind="ExternalOutput").ap(), name)
        self.outs.append(t)
        return t

    def dscr(self, name, shape, dt):
        return TT(self.nc.dram_tensor(name, list(shape), dt, kind="Internal").ap(), name)

    def op(self, *a, **k):
        return self.S.op(*a, **k)

    def dma(self, q, out, in_, reads, writes, **kw):
        return self.S.op(q, lambda e: e.dma_start(out=out, in_=in_, **kw), reads=reads, writes=writes, is_dma=True)

    def finish(self):
        deps = []
        for t in self.outs:
            deps.append(t.d)
            deps.extend(t._sub.values())
        self.S.op("sp", lambda e: e.nop(), reads=deps)
        self.S.emit()
        return self.nc


def make_tables(pb, modP, ngP, KC):
    mod = pb.sb("mod_sb", [128, 6, KC, 2], F32)
    ng = pb.sb("ng_sb", [128, 4, KC], F32)
    pb.dma("sp", mod[:], modP[:], [modP.d], [mod.d])
    pb.dma("sp", ng[:], ngP[:], [ngP.d], [ng.d])
    tabs = {}
    for si, sub in enumerate(("m", "f")):
        for wi, who in enumerate(("x", "c")):
            gsc = pb.sb("gsc_%s%s" % (sub, who), [128, KC], F32)
            sh = pb.sb("sh_%s%s" % (sub, who), [128, KC], F32)
            gg = pb.sb("gg_%s%s" % (sub, who), [128, KC], F32)
            pb.op("dve", lambda e, gsc=gsc, si=si, wi=wi: e.scalar_tensor_tensor(
                out=gsc[:], in0=mod[:, 3 * si + 1, :, wi], scalar=1.0, in1=ng[:, 2 * si, :],
                op0=ALU.add, op1=ALU.mult), reads=[mod.d, ng.d], writes=[gsc.d])
            pb.op("dve", lambda e, sh=sh, si=si, wi=wi: e.tensor_copy(out=sh[:], in_=mod[:, 3 * si, :, wi]),
                  reads=[mod.d], writes=[sh.d])
            pb.op("dve", lambda e, gg=gg, si=si, wi=wi: e.tensor_tensor(
                out=gg[:], in0=mod[:, 3 * si + 2, :, wi], in1=ng[:, 2 * si + 1, :], op=ALU.mult),
                reads=[mod.d, ng.d], writes=[gg.d])
            tabs[(sub, who)] = (gsc, sh, gg)
    return tabs


class Common:
    def __init__(self, pb, T, TW):
        self.pb = pb
        self.T = T
        self.TW = TW
        self.ones = pb.sb("ones_f", [128, 128], F32)
        pb.op("dve", lambda e: e.memset(self.ones[:], 1.0), writes=[self.ones.d])
        self.ps_stat = pb.ps("ps_stat")
        self.xst = [pb.sb("xst%d" % i, [128, T], F32) for i in range(2)]
        self.yst = [pb.sb("yst%d" % i, [128, T], F32) for i in range(2)]
        self.tmp = self.yst
        self.epsb = pb.sb("epsb", [128, 1], F32)
        pb.op("dve", lambda e: e.memset(self.epsb[:], EPS), writes=[self.epsb.d])
        self.ssq = pb.sb("ssq", [128, T], F32)
        self._i = 0


def finalize_stats(pb, cm, D, rstd):
    T, TW = cm.T, cm.TW
    for i in range(T // TW):
        sl = slice(i * TW, (i + 1) * TW)
        pb.op("pe", lambda e, sl=sl: e.matmul(cm.ps_stat[:, 0:TW], cm.ones[:], cm.ssq[:, sl], start=True, stop=True),
              reads=[cm.ones.d, cm.ssq.d], writes=[cm.ps_stat.d])
        pb.op("act", lambda e, sl=sl: e.activation(out=rstd[:, sl], in_=cm.ps_stat[:, 0:TW], func=AF.Sqrt,
                                                   scale=1.0 / D, bias=cm.epsb[:, 0:1]),
              reads=[cm.ps_stat.d, cm.epsb.d], writes=[rstd.d])
    pb.op("dve", lambda e: e.reciprocal(out=rstd[:], in_=rstd[:]), reads=[rstd.d], writes=[rstd.d])


def stats_pass(pb, cm, xT, KC, D, rstd):
    T = cm.T
    for kc in range(KC):
        xs = cm.xst[kc % 2]
        pb.dma("sp", xs[:], xT[kc * 128:(kc + 1) * 128, :], [xT.dk(kc)], [xs.d])
        if kc == 0:
            pb.op("act", lambda e, xs=xs: e.activation(out=cm.ssq[:], in_=xs[:], func=AF.Square),
                  reads=[xs.d], writes=[cm.ssq.d])
        else:
            tm = cm.tmp[kc % 2]
            pb.op("act", lambda e, xs=xs, tm=tm: e.activation(out=tm[:], in_=xs[:], func=AF.Square),
                  reads=[xs.d], writes=[tm.d])
            pb.op("dve", lambda e, tm=tm: e.tensor_tensor(out=cm.ssq[:], in0=cm.ssq[:], in1=tm[:], op=ALU.add),
                  reads=[tm.d, cm.ssq.d], writes=[cm.ssq.d])
    finalize_stats(pb, cm, D, rstd)


def col_groups(t0, t1, NCTX):
    g = []
    if t0 < NCTX:
        g.append(("c", t0, min(t1, NCTX)))
    if t1 > NCTX:
        g.append(("x", max(t0, NCTX), t1))
    return g


def modulate_pass(pb, cm, xT, rstd, tabs, sub, xres, t0, Tres, KC, NCTX):
    for kc in range(KC):
        xs = cm.xst[kc % 2]
        tm = cm.tmp[kc % 2]
        pb.dma("sp", xs[:, 0:Tres], xT[kc * 128:(kc + 1) * 128, t0:t0 + Tres], [xT.dk(kc)], [xs.d])
        for who, a, b in col_groups(t0, t0 + Tres, NCTX):
            gsc, sh, gg = tabs[(sub, who)]
            pb.op("dve", lambda e, xs=xs, tm=tm, a=a, b=b, gsc=gsc, kc=kc: e.scalar_tensor_tensor(
                out=tm[:, a - t0:b - t0], in0=xs[:, a - t0:b - t0], scalar=gsc[:, kc:kc + 1], in1=rstd[:, a:b],
                op0=ALU.mult, op1=ALU.mult), reads=[xs.d, gsc.d, rstd.d], writes=[tm.d])
            pb.op("act", lambda e, tm=tm, a=a, b=b, sh=sh, kc=kc: e.activation(
                out=xres[:, kc, a - t0:b - t0], in_=tm[:, a - t0:b - t0], func=AF.Identity, bias=sh[:, kc:kc + 1]),
                reads=[tm.d, sh.d], writes=[xres.d])


def residual_pass(pb, cm, yT, xTin, xTout, rstd, tabs, sub, KC, NCTX, accumulate_ssq=False):
    T = cm.T
    for kc in range(KC):
        xs = cm.xst[kc % 2]
        ys = cm.yst[kc % 2]
        tm = ys
        pb.dma("sp", xs[:], xTin[kc * 128:(kc + 1) * 128, :], [xTin.dk(kc)], [xs.d])
        pb.dma("sp", ys[:], yT[kc * 128:(kc + 1) * 128, :], [yT.dk(kc)], [ys.d])
        for who, a, b in col_groups(0, T, NCTX):
            gsc, sh, gg = tabs[(sub, who)]
            pb.op("dve", lambda e, ys=ys, a=a, b=b, gg=gg, kc=kc: e.scalar_tensor_tensor(
                out=ys[:, a:b], in0=ys[:, a:b], scalar=gg[:, kc:kc + 1], in1=rstd[:, a:b],
                op0=ALU.mult, op1=ALU.mult), reads=[ys.d, gg.d, rstd.d], writes=[ys.d])
        pb.op("pool", lambda e, xs=xs, ys=ys: e.tensor_tensor(out=xs[:], in0=xs[:], in1=ys[:], op=ALU.add),
              reads=[xs.d, ys.d], writes=[xs.d])
        pb.dma("sp", xTout[kc * 128:(kc + 1) * 128, :], xs[:], [xs.d], [xTout.dk(kc)])
        if accumulate_ssq:
            if kc == 0:
                pb.op("act", lambda e, xs=xs: e.activation(out=cm.ssq[:], in_=xs[:], func=AF.Square),
                      reads=[xs.d], writes=[cm.ssq.d])
            else:
                pb.op("act", lambda e, xs=xs, tm=tm: e.activation(out=tm[:], in_=xs[:], func=AF.Square),
                      reads=[xs.d], writes=[tm.d])
                pb.op("dve", lambda e, tm=tm: e.tensor_tensor(out=cm.ssq[:], in0=cm.ssq[:], in1=tm[:], op=ALU.add),
                      reads=[tm.d, cm.ssq.d], writes=[cm.ssq.d])


class WStream:
    def __init__(self, pb, KCmax, nbuf=4, nstage=2):
        self.pb = pb
        self.stage = [pb.sb("wstage%d" % i, [128, KCmax, 128], F32) for i in range(nstage)]
        self.slab = [pb.sb("wslab%d" % i, [128, KCmax, 128], BF16) for i in range(nbuf)]
        self.i = 0

    def fetch(self, wap, KC, wdep):
        pb = self.pb
        st = self.stage[self.i % len(self.stage)]
        sl = self.slab[self.i % len(self.slab)]
        self.i += 1
        src = wap.rearrange("(c p) n -> p c n", p=128)
        nsp = max(1, KC // 8)
        step = KC // nsp

        def f(e, st=st, src=src):
            return [e.dma_start(out=st[:, j * step:(j + 1) * step, :], in_=src[:, j * step:(j + 1) * step, :]) for j in range(nsp)]
        pb.op("sp", f, reads=[wdep], writes=[st.d], is_dma=True, n_inst=nsp)
        pb.op("pool", lambda e, st=st, sl=sl: e.tensor_copy(out=sl[:, 0:KC, :], in_=st[:, 0:KC, :]),
              reads=[st.d], writes=[sl.d])
        return sl


def linear(pb, ws, xres, KC, wfn, wdep, groups, tiles, psums, epi, prefetch=1, KS=1):
    G = len(groups[0])
    slabs = {}

    def fetch_group(gi):
        if KS == 1:
            slabs[gi] = [[ws.fetch(wfn(nb), KC, wdep)] for nb in groups[gi]]
        else:
            slabs[gi] = [[ws.fetch(wfn(nb, s_), KC, wdep) for s_ in range(KS)] for nb in groups[gi]]
    for gi in range(min(prefetch, len(groups))):
        fetch_group(gi)
    cnt = 0
    for gi, grp in enumerate(groups):
        if gi + prefetch < len(groups):
            fetch_group(gi + prefetch)
        sl = slabs.pop(gi)
        for ti, (c0, cw) in enumerate(tiles):
            pss = [psums[(cnt % 2) * G + m] for m in range(G)]
            cnt += 1
            for m in range(G):
                for s_ in range(KS):
                    for kc in range(KC):
                        pb.op("pe", lambda e, kc=kc, s_=s_, ps=pss[m], s=sl[m][s_], c0=c0, cw=cw: e.matmul(
                            ps[:, 0:cw], s[:, kc, :], xres[:, s_ * KC + kc, c0:c0 + cw],
                            start=(kc == 0 and s_ == 0), stop=(kc == KC - 1 and s_ == KS - 1)),
                            reads=[sl[m][s_].d, xres.d], writes=[pss[m].d])
            epi(gi, grp, ti, (c0, cw), pss)


def s5_consts(NK):
    jc = np.arange(128)
    j = (jc // 16).astype(np.float32)
    consts = np.zeros((128, 8, 128), np.float32)
    consts[:, 0, :] = j[None, :]
    consts[:, 1, :] = j[None, :] + 1
    consts[:, 2, :] = -j[None, :]
    consts[:, 3, :] = 8 - j[None, :]
    consts[:, 4, :] = (j[:, None] <= j[None, :])
    consts[:, 5, :] = (j[:, None] >= j[None, :])
    consts[:, 6, :] = np.eye(128)
    cpart = np.zeros((128, 4), np.float32)
    cpart[:, 0] = 7 - j
    cpart[:, 1] = j
    cpart[:64, 2] = 1.0
    cpart[64:, 3] = 1.0
    ktab = np.broadcast_to(np.arange(NK, dtype=np.float32)[None, :], (128, NK)).copy()
    return consts, cpart, ktab


def s5_params(a_re, a_im, log_dt, b_re, b_im, c_re, c_im, dskip, groups):
    NG = len(groups)
    NP = NG // 2
    pn_sc = np.zeros((NP, 2, 128, 3), np.float32)
    pn_bc = np.zeros((NP, 2, 128, 4, 16), np.float32)
    pj_sc = np.zeros((NG, 2, 128, 2, 64), np.float32)
    pj_dt = np.zeros((NG, 2, 128, 1), np.float32)
    pj_b = np.zeros((NG, 2, 128, 2, 64), np.float32)
    pj_d = np.zeros((NG, 128, 1), np.float32)
    for gi, g in enumerate(groups):
        p, gl = gi // 2, gi % 2
        rows = slice(64 * gl, 64 * gl + 64)
        for d in range(2):
            pn_sc[p, d, rows, 0] = a_re[d, g]
            pn_sc[p, d, rows, 1] = a_im[d, g]
            pn_sc[p, d, rows, 2] = log_dt[d, g]
            pn_bc[p, d, rows, 0] = b_re[d, g]
            pn_bc[p, d, rows, 1] = b_im[d, g]
            pn_bc[p, d, rows, 2] = c_re[d, g].T
            pn_bc[p, d, rows, 3] = c_im[d, g].T
            pj_sc[gi, d, :, 0, :] = a_re[d, g][None, :]
            pj_sc[gi, d, :, 1, :] = a_im[d, g][None, :]
            pj_dt[gi, d, :, 0] = log_dt[d, g]
            pj_b[gi, d, :, 0, :] = np.tile(b_re[d, g].T, (8, 1))
            pj_b[gi, d, :, 1, :] = np.tile(b_im[d, g].T, (8, 1))
        pj_d[gi, :, 0] = np.tile(dskip[16 * g:16 * g + 16], 8)
    return dict(pn_sc=pn_sc, pn_bc=pn_bc, pj_sc=pj_sc, pj_dt=pj_dt, pj_b=pj_b, pj_d=pj_d)


GRID_W = 64
NEG = -30000.0


def na_bias_tables(rpb, ci, rows_per_core=32, rows_total=256, NQT=16, NEXT=20):
    H = rpb.shape[0]
    tab = np.full((H, 128, 5, 768), NEG, np.float32)
    q = np.arange(128)
    rp, qc = q // 64, q % 64
    kk = np.arange(128)
    kp, kcol = kk // 64, kk % 64
    col_start = np.clip(qc - 8, 0, GRID_W - 16)
    for cl in range(5):
        i = [0, 1, 2, NQT - 2, NQT - 1][cl]
        lo = 0 if cl == 0 else (NEXT - 6 if cl == 4 else i)
        n = 6 if cl in (0, 4) else 5
        qr = rows_per_core * ci + 2 * i + rp
        r0 = np.clip(qr - 4, 0, rows_total - 8)
        for m in range(n):
            lc = lo + m - 2
            gr = rows_per_core * ci + 2 * lc + kp
            valid = ((gr[None, :] >= 0) & (gr[None, :] < rows_total) & (gr[None, :] >= r0[:, None]) & (gr[None, :] < r0[:, None] + 8)
                     & (kcol[None, :] >= col_start[:, None]) & (kcol[None, :] < col_start[:, None] + 16))
            rr = np.clip(gr[None, :] - qr[:, None] + 7, 0, 14)
            cc = np.clip(kcol[None, :] - qc[:, None] + 15, 0, 30)
            vals = rpb[:, rr, cc]
            tab[:, :, cl, m * 128:(m + 1) * 128] = np.where(valid[None], vals, NEG)
    return tab


def build_progA(D, T, NCTX):
    pb = PB()
    KC = D // 128
    KL = T // 8
    G = D // 16
    xT = pb.din("xT", [D, T], F32)
    modP = pb.din("modP", [128, 6, KC, 2], F32)
    ngP = pb.din("ngP", [128, 4, KC], F32)
    Xloc = pb.dout("Xloc", [G, 8, 16, KL], BF16)
    tabs = make_tables(pb, modP, ngP, KC)
    cm = Common(pb, T, 260 if T % 260 == 0 else T)
    rstd = pb.sb("rstd", [128, T], F32)
    hb = [pb.sb("hb%d" % i, [128, T], BF16) for i in range(2)]
    stats_pass(pb, cm, xT, KC, D, rstd)
    for kc in range(KC):
        xs = cm.xst[kc % 2]
        tm = cm.tmp[kc % 2]
        h = hb[kc % 2]
        pb.dma("sp", xs[:], xT[kc * 128:(kc + 1) * 128, :], [xT.dk(kc)], [xs.d])
        for who, a, b in col_groups(0, T, NCTX):
            gsc, sh, gg = tabs[("m", who)]
            pb.op("dve", lambda e, xs=xs, tm=tm, a=a, b=b, gsc=gsc, kc=kc: e.scalar_tensor_tensor(
                out=tm[:, a:b], in0=xs[:, a:b], scalar=gsc[:, kc:kc + 1], in1=rstd[:, a:b],
                op0=ALU.mult, op1=ALU.mult), reads=[xs.d, gsc.d, rstd.d], writes=[tm.d])
            pb.op("act", lambda e, tm=tm, h=h, a=a, b=b, sh=sh, kc=kc: e.activation(
                out=h[:, :].rearrange("p (j k) -> p k j", j=8)[:, a // 8:b // 8, :],
                in_=tm[:, a:b].rearrange("p (k j) -> p k j", j=8), func=AF.Identity, bias=sh[:, kc:kc + 1]),
                reads=[tm.d, sh.d], writes=[h.d])

        def f(e, h=h, kc=kc):
            return [e.dma_start(out=Xloc[8 * kc + gl].rearrange("j c k -> c j k"),
                                in_=h[16 * gl:16 * gl + 16, :].rearrange("c (j k) -> c j k", j=8)) for gl in range(8)]
        pb.op("act", f, reads=[h.d], writes=[Xloc.dk(kc)], is_dma=True, n_inst=8)
    return pb.finish()


def build_progP(D, NB):
    pb = PB()
    KC = D // 128
    condP = pb.din("condP", [128, KC, 2], F32)
    wP = pb.din("wP", [D, NB * 128], F32)
    bP = pb.din("bP", [128, NB], F32)
    out = pb.dout("modT", [128, NB, 2], F32)
    cs = pb.sb("cs", [128, KC, 2], F32)
    xres = pb.sb("xres", [128, KC, 2], BF16)
    bs = pb.sb("bs", [128, NB], F32)
    ob = pb.sb("ob", [128, NB, 2], F32)
    pb.dma("act", cs[:], condP[:], [condP.d], [cs.d])
    pb.dma("act", bs[:], bP[:], [bP.d], [bs.d])
    pb.op("act", lambda e: e.activation(out=xres[:], in_=cs[:], func=AF.Silu), reads=[cs.d], writes=[xres.d])
    ws = WStream(pb, KC, nbuf=3, nstage=3)
    psums = [pb.ps("psl%d" % i) for i in range(2)]

    def epi(gi, grp, ti, cc, pss):
        nb = grp[0]
        pb.op("act", lambda e: e.activation(out=ob[:, nb, :], in_=pss[0][:, 0:2], func=AF.Identity, bias=bs[:, nb:nb + 1]),
              reads=[pss[0].d, bs.d], writes=[ob.d])
    linear(pb, ws, xres, KC, lambda nb: wP[:, nb * 128:(nb + 1) * 128], wP.d, [(j,) for j in range(NB)], [(0, 2)], psums, epi, prefetch=2)
    pb.dma("sp", out[:], ob[:], [ob.d], [out.d])
    return pb.finish()


import os

TWO_PI = 2.0 * math.pi


class EW:
    def __init__(self, pb):
        self.pb = pb
        self.rr = 0

    def eng(self):
        self.rr += 1
        return "dve" if self.rr % 2 else "pool"

    def tt(self, o, a, b, op, eng=None):
        eng = eng or self.eng()
        self.pb.op(eng, lambda e: e.tensor_tensor(out=o[1], in0=a[1], in1=b[1], op=op),
                   reads=[a[0].d, b[0].d], writes=[o[0].d])

    def ts(self, o, a, s1, op0, s2=None, op1=None, eng="dve"):
        reads = [a[0].d]
        v1 = s1
        if isinstance(s1, tuple):
            reads.append(s1[0].d); v1 = s1[1]
        v2 = s2
        if isinstance(s2, tuple):
            reads.append(s2[0].d); v2 = s2[1]
        if op1 is None:
            self.pb.op(eng, lambda e: e.tensor_scalar(out=o[1], in0=a[1], scalar1=v1, scalar2=None, op0=op0),
                       reads=reads, writes=[o[0].d])
        else:
            self.pb.op(eng, lambda e: e.tensor_scalar(out=o[1], in0=a[1], scalar1=v1, scalar2=v2, op0=op0, op1=op1),
                       reads=reads, writes=[o[0].d])

    def act(self, o, a, func, scale=1.0, bias=None):
        reads = [a[0].d]
        sc = scale
        if isinstance(scale, tuple):
            reads.append(scale[0].d); sc = scale[1]
        kw = {}
        if bias is not None:
            reads.append(bias[0].d); kw["bias"] = bias[1]
        self.pb.op("act", lambda e: e.activation(out=o[1], in_=a[1], func=func, scale=sc, **kw),
                   reads=reads, writes=[o[0].d])

    def copy(self, o, a, eng="dve"):
        self.pb.op(eng, lambda e: e.tensor_copy(out=o[1], in_=a[1]), reads=[a[0].d], writes=[o[0].d])

    def frac(self, o, a, ti, tf):
        self.copy(ti, a, "dve")
        self.copy(tf, ti, "dve")
        self.tt(o, a, tf, ALU.subtract, "dve")

    def cmul(self, o_re, o_im, a_re, a_im, b_re, b_im, t1, t2):
        self.tt(t1, a_re, b_re, ALU.mult)
        self.tt(t2, a_im, b_im, ALU.mult)
        self.tt(o_re, t1, t2, ALU.subtract)
        self.tt(t1, a_re, b_im, ALU.mult)
        self.tt(t2, a_im, b_re, ALU.mult)
        self.tt(o_im, t1, t2, ALU.add)


SIN_SCALE = TWO_PI * (1.0 - 2e-6)


def build_progB(NP, NCK, NXT, XW, stop=99):
    pb = PB()
    ew = EW(pb)
    NG = 2 * NP
    NXK = NXT * XW
    NK = NCK + NXK
    Xg = pb.din("Xg", [NG, 128, NK], BF16)
    pn_sc = pb.din("pn_sc", [NP, 2, 128, 3], F32)
    pn_bc = pb.din("pn_bc", [NP, 2, 128, 4, 16], F32)
    pj_sc = pb.din("pj_sc", [NG, 2, 128, 2, 64], F32)
    pj_dt = pb.din("pj_dt", [NG, 2, 128, 1], F32)
    pj_b = pb.din("pj_b", [NG, 2, 128, 2, 64], F32)
    pj_d = pb.din("pj_d", [NG, 128, 1], F32)
    consts = pb.din("consts", [128, 8, 128], F32)
    cpart = pb.din("cpart", [128, 4], F32)
    ktab_d = pb.din("ktab", [128, NK], F32)
    Yg = pb.dout("Yg", [NG, 128, NK], BF16)

    cst = pb.sb("cst", [128, 8, 128], F32)
    cpt = pb.sb("cpt", [128, 4], F32)
    ktab = pb.sb("ktabs", [128, NK], F32)
    pb.dma("sp", cst[:], consts[:], [consts.d], [cst.d])
    pb.dma("sp", cpt[:], cpart[:], [cpart.d], [cpt.d])
    pb.dma("sp", ktab[:], ktab_d[:], [ktab_d.d], [ktab.d])

    def T(name, shape, dt=F32):
        return pb.sb(name, shape, dt)
    tA = [T("tA%d" % i, [128, 128]) for i in range(10)]
    tI = T("tI", [128, 128], I32)
    cA = [T("cA%d" % i, [128, 8]) for i in range(1)]
    cI = T("cI", [128, 8], I32)
    psn = T("psn", [128, 3]); pbc = T("pbc", [128, 4, 16])
    pjs = T("pjs", [128, 2, 2, 64]); pjd = T("pjd", [128, 2, 1]); pjb = T("pjb", [128, 2, 2, 64]); pdd = T("pdd", [128, 2, 1])
    col = T("col", [128, 16])
    colj = T("colj", [128, 4])
    W1 = T("W1", [128, 2, 2, 2, 128], BF16)
    Wsum = T("Wsum", [128, 2, 128], BF16)
    Wint = T("Wint", [128, 2, 2, 128], F32)
    Wintz = T("Wintz", [128, 2, 2, 2, 128], BF16)
    QQz = T("QQz", [128, 2, 2, 128], BF16)
    PT = T("PT", [128, 2, 128], BF16)
    QQ = T("QQ", [128, 2, 128], BF16)
    cosT = T("cosT", [128, NK]); sinT = T("sinT", [128, NK])
    Sre = T("Sre", [128, NK]); Sim = T("Sim", [128, NK])
    b1 = T("b1", [128, NK]); b2 = T("b2", [128, NK]); b3 = T("b3", [128, NK])
    bI = T("bI", [128, NK], I32)
    Hb = [[T("Hb%d%d" % (d, r), [128, NK + 2], BF16) for r in range(2)] for d in range(2)]
    Xs = [[T("Xs%d%d" % (i, g), [128, NK], BF16) for g in range(2)] for i in range(2)]
    Yb = [T("Yb%d" % i, [128, NK], BF16) for i in range(2)]
    ps_s = [pb.ps("ps_s%d" % i) for i in range(4)]
    ps_y = [pb.ps("ps_y%d" % i) for i in range(2)]
    ps_w = pb.ps("ps_w")
    for d in range(2):
        for r in range(2):
            pb.op("pool", lambda e, d=d, r=r: e.memset(Hb[d][r][:], 0.0), writes=[Hb[d][r].d])
    pb.op("pool", lambda e: e.memset(W1[:], 0.0), writes=[W1.d])

    if False:
        for t_ in tA + [col, colj, cosT, sinT, Sre, Sim, b1, b2, b3, PT, QQ, Wint, Wintz, QQz, Wsum, psn, pbc, pjs, pjd, pjb, pdd] + Yb + Xs[0] + Xs[1]:
            pb.op("dve", lambda e, t_=t_: e.memset(t_[:], 0.0), writes=[t_.d])
    def V(t, *idx):
        return (t, t.t[idx] if idx else t.t[:])

    def cpow(o_re, o_im, tile_r, col_r, tile_t, col_t, shape_idx):
        si = shape_idx
        mag, tt_, fr, sn = [(tA[i], tA[i].t[si]) for i in range(4)]
        ti = (tI, tI.t[si])
        tf = (tA[4], tA[4].t[si])
        ew.act(mag, tile_r, AF.Exp, scale=col_r)
        ew.ts(tt_, tile_t, col_t, ALU.mult)
        ew.frac(fr, tt_, ti, tf)
        ew.act(sn, fr, AF.Sin, scale=SIN_SCALE)
        ew.tt(o_im, mag, sn, ALU.mult)
        ew.ts(tt_, tt_, 0.25, ALU.add)
        ew.frac(fr, tt_, ti, tf)
        ew.act(sn, fr, AF.Sin, scale=SIN_SCALE)
        ew.tt(o_re, mag, sn, ALU.mult)

    tiles_nat = [(0, NCK)] + [(NCK + i * XW, XW) for i in range(NXT)]

    def d1col(c0):
        return c0 - NCK if c0 >= NCK else NXK + c0

    for p in range(NP):
        xs = Xs[p % 2]
        for g in range(2):
            pb.dma("act", xs[g][:], Xg[2 * p + g], [Xg.d], [xs[g].d])
        for d in range(2):
            pb.dma("sp", psn[:], pn_sc[p, d], [pn_sc.d], [psn.d])
            pb.dma("sp", pbc[:], pn_bc[p, d], [pn_bc.d], [pbc.d])
            for g in range(2):
                pb.dma("sp", pjs[:, g], pj_sc[2 * p + g, d], [pj_sc.d], [pjs.d])
                pb.dma("sp", pjd[:, g], pj_dt[2 * p + g, d], [pj_dt.d], [pjd.d])
                pb.dma("sp", pjb[:, g], pj_b[2 * p + g, d], [pj_b.d], [pjb.d])
            c = lambda i: (col, col.t[:, i:i + 1])
            ew.act(c(0), V(psn, slice(None), slice(2, 3)), AF.Exp)
            ew.tt(c(1), V(psn, slice(None), slice(0, 1)), c(0), ALU.mult, "dve")
            ew.tt(c(2), V(psn, slice(None), slice(1, 2)), c(0), ALU.mult, "dve")
            ew.ts(c(2), c(2), 1.0 / TWO_PI, ALU.mult)
            s1 = (slice(None), slice(0, 1))
            ew.act((tA[0], tA[0].t[s1]), c(1), AF.Exp)
            ew.frac((tA[1], tA[1].t[s1]), c(2), (tI, tI.t[s1]), (tA[4], tA[4].t[s1]))
            ew.act((tA[2], tA[2].t[s1]), (tA[1], tA[1].t[s1]), AF.Sin, scale=SIN_SCALE)
            ew.tt(c(4), (tA[0], tA[0].t[s1]), (tA[2], tA[2].t[s1]), ALU.mult, "dve")
            ew.ts((tA[3], tA[3].t[s1]), c(2), 0.25, ALU.add)
            ew.frac((tA[1], tA[1].t[s1]), (tA[3], tA[3].t[s1]), (tI, tI.t[s1]), (tA[4], tA[4].t[s1]))
            ew.act((tA[2], tA[2].t[s1]), (tA[1], tA[1].t[s1]), AF.Sin, scale=SIN_SCALE)
            ew.tt(c(3), (tA[0], tA[0].t[s1]), (tA[2], tA[2].t[s1]), ALU.mult, "dve")
            lr = V(psn, slice(None), slice(0, 1)); li = V(psn, slice(None), slice(1, 2))
            q = lambda i: (tA[5], tA[5].t[:, i:i + 1])
            ew.ts(q(0), c(3), -1.0, ALU.add)
            ew.tt(q(1), lr, lr, ALU.mult, "dve"); ew.tt(q(2), li, li, ALU.mult, "dve")
            ew.tt(q(1), q(1), q(2), ALU.add, "dve")
            pb.op("dve", lambda e: e.reciprocal(out=tA[5].t[:, 1:2], in_=tA[5].t[:, 1:2]), reads=[tA[5].d], writes=[tA[5].d])
            ew.tt(q(2), q(0), lr, ALU.mult, "dve"); ew.tt(q(3), c(4), li, ALU.mult, "dve"); ew.tt(q(2), q(2), q(3), ALU.add, "dve")
            ew.tt(q(3), c(4), lr, ALU.mult, "dve"); ew.tt(q(4), q(0), li, ALU.mult, "dve"); ew.tt(q(3), q(3), q(4), ALU.subtract, "dve")
            ew.tt(c(5), q(2), q(1), ALU.mult, "dve"); ew.tt(c(6), q(3), q(1), ALU.mult, "dve")
            if True:
                ew.act(c(7), c(1), AF.Exp, scale=8.0)
                ew.ts(q(5), c(2), 8.0, ALU.mult)
                ew.frac(c(8), q(5), (tI, tI.t[s1]), (tA[4], tA[4].t[s1]))
            if stop <= 1:
                return pb.finish()
            bbr = (tA[6], tA[6].t[:, 0:16]); bbi = (tA[6], tA[6].t[:, 16:32])
            t1 = (tA[7], tA[7].t[:, 0:16]); t2 = (tA[7], tA[7].t[:, 16:32])
            Br = V(pbc, slice(None), 0); Bi = V(pbc, slice(None), 1)
            ew.ts(t1, Br, c(5), ALU.mult); ew.ts(t2, Bi, c(6), ALU.mult); ew.tt(bbr, t1, t2, ALU.subtract)
            ew.ts(t1, Br, c(6), ALU.mult); ew.ts(t2, Bi, c(5), ALU.mult); ew.tt(bbi, t1, t2, ALU.add)
            full = (slice(None), slice(None))
            Ctr = pbc.t[:, 2, :]; Cti = pbc.t[:, 3, :]

            def bc16(ap):
                return ap.unsqueeze(1).to_broadcast([128, 8, 16])

            def v3(t):
                return t.t[:].rearrange("p (j c) -> p j c", c=16)
            eQ, eW, eP = (0, 1, 2) if d == 0 else (2, 3, 0)
            pw_re = (tA[8], tA[8].t[:]); pw_im = (tA[9], tA[9].t[:])
            if stop <= 2:
                return pb.finish()
            cpow(pw_re, pw_im, (cst, cst.t[:, eQ, :]), c(1), (cst, cst.t[:, eQ, :]), c(2), full)

            def cmul_bc(o_re_ap, o_im_ap, o_t, xr_ap, xi_ap, x_t, neg_im=False):
                t1_ = (tA[5], v3(tA[5])); t2_ = (tA[7], v3(tA[7]))
                pr = (tA[8], v3(tA[8])); pi_ = (tA[9], v3(tA[9]))
                ew.tt(t1_, pr, (x_t, bc16(xr_ap)), ALU.mult); ew.tt(t2_, pi_, (x_t, bc16(xi_ap)), ALU.mult)
                ew.tt((o_t, o_re_ap), t1_, t2_, ALU.subtract)
                ew.tt(t1_, pr, (x_t, bc16(xi_ap)), ALU.mult); ew.tt(t2_, pi_, (x_t, bc16(xr_ap)), ALU.mult)
                if neg_im:
                    ew.tt(t1_, t1_, t2_, ALU.add)
                    ew.ts((o_t, o_im_ap), t1_, -1.0, ALU.mult)
                else:
                    ew.tt((o_t, o_im_ap), t1_, t2_, ALU.add)
            r3 = lambda t, i: t.t[:, i, :].rearrange("p (j c) -> p j c", c=16)
            cmul_bc(r3(QQ, 0), r3(QQ, 1), QQ, Ctr, Cti, pbc)
            if stop <= 3:
                return pb.finish()
            cpow(pw_re, pw_im, (cst, cst.t[:, eW, :]), c(1), (cst, cst.t[:, eW, :]), c(2), full)
            cmul_bc(Wint.t[:, d, 0, :].rearrange("p (j c) -> p j c", c=16),
                    Wint.t[:, d, 1, :].rearrange("p (j c) -> p j c", c=16), Wint, Ctr, Cti, pbc, neg_im=True)
            cpow(pw_re, pw_im, (cst, cst.t[:, eP, :]), c(1), (cst, cst.t[:, eP, :]), c(2), full)
            cmul_bc(r3(PT, 0), r3(PT, 1), PT, tA[6].t[:, 0:16], tA[6].t[:, 16:32], tA[6], neg_im=True)
            if stop <= 4:
                return pb.finish()
            for g in range(2):
                rm = (cpt, cpt.t[:, 2 + g:3 + g])
                for r in range(2):
                    ew.ts((QQz, QQz.t[:, g, r, :]), (QQ, QQ.t[:, r, :]), rm, ALU.mult)
                    ew.ts((Wintz, Wintz.t[:, g, d, r, :]), (Wint, Wint.t[:, d, r, :]), rm, ALU.mult)
                oc = (2 * g + d) * 128
                pb.op("pe", lambda e, g=g, oc=oc: e.matmul(ps_w[:, oc:oc + 128], PT[:, 0, :], QQz[:, g, 0, :], start=True, stop=False),
                      reads=[PT.d, QQz.d], writes=[ps_w.d])
                pb.op("pe", lambda e, g=g, oc=oc: e.matmul(ps_w[:, oc:oc + 128], PT[:, 1, :], QQz[:, g, 1, :], start=False, stop=True),
                      reads=[PT.d, QQz.d], writes=[ps_w.d])
            if stop <= 5:
                return pb.finish()
            j2 = (slice(None), slice(0, 128))
            cj = lambda i: (colj, colj.t[:, i:i + 1])
            for g in range(2):
                ar = V(pjs, slice(None), g, 0); ai = V(pjs, slice(None), g, 1)
                s64 = (slice(None), slice(0, 64))
                u = lambda i: (tA[i], tA[i].t[s64])
                ew.act(cj(0), V(pjd, slice(None), g), AF.Exp)
                ew.ts(u(5), ar, cj(0), ALU.mult)
                ew.ts(u(6), ai, cj(0), ALU.mult, 1.0 / TWO_PI, ALU.mult)
                ones_col = (cpt, cpt.t[:, 2:3])
                cpow(u(8), u(9), u(5), 1.0, u(6), 1.0, s64)
                k0 = (tA[7], tA[7].t[:, 0:64]); k1 = (tA[7], tA[7].t[:, 64:128])
                m0 = (tA[0], tA[0].t[s64]); m1 = (tA[1], tA[1].t[s64]); m2 = (tA[2], tA[2].t[s64])
                ew.ts(u(8), u(8), -1.0, ALU.add)
                ew.tt(m0, ar, ar, ALU.mult); ew.tt(m1, ai, ai, ALU.mult); ew.tt(m0, m0, m1, ALU.add)
                pb.op("dve", lambda e: e.reciprocal(out=tA[0].t[s64], in_=tA[0].t[s64]), reads=[tA[0].d], writes=[tA[0].d])
                ew.tt(m1, u(8), ar, ALU.mult); ew.tt(m2, u(9), ai, ALU.mult); ew.tt(m1, m1, m2, ALU.add); ew.tt(k0, m1, m0, ALU.mult)
                ew.tt(m1, u(9), ar, ALU.mult); ew.tt(m2, u(8), ai, ALU.mult); ew.tt(m1, m1, m2, ALU.subtract); ew.tt(k1, m1, m0, ALU.mult)
                Bjr = V(pjb, slice(None), g, 0); Bji = V(pjb, slice(None), g, 1)
                ew.tt(m1, k0, Bjr, ALU.mult); ew.tt(m2, k1, Bji, ALU.mult); ew.tt(u(8), m1, m2, ALU.subtract)
                ew.tt(m1, k0, Bji, ALU.mult); ew.tt(m2, k1, Bjr, ALU.mult); ew.tt(u(9), m1, m2, ALU.add)
                ecol = (cpt, cpt.t[:, d:d + 1])
                pwr = (tA[5], tA[5].t[:, 64:128]); pwi = (tA[6], tA[6].t[:, 64:128])
                cpow(pwr, pwi, u(5), ecol, u(6), ecol, s64)
                ew.tt(m1, pwr, u(8), ALU.mult); ew.tt(m2, pwi, u(9), ALU.mult)
                ew.tt((W1, W1.t[:, d, g, 0, 64 * g:64 * g + 64]), m1, m2, ALU.subtract)
                ew.tt(m1, pwr, u(9), ALU.mult); ew.tt(m2, pwi, u(8), ALU.mult)
                ew.tt((W1, W1.t[:, d, g, 1, 64 * g:64 * g + 64]), m1, m2, ALU.add)
            if stop <= 6:
                return pb.finish()
            order = tiles_nat if d == 0 else (tiles_nat[1:] + tiles_nat[:1])
            for ti, (c0, cw) in enumerate(order):
                o0 = c0 if d == 0 else d1col(c0)
                for r in range(2):
                    ps = ps_s[(ti % 2) * 2 + r]
                    for g in range(2):
                        pb.op("pe", lambda e, ps=ps, g=g, r=r, c0=c0, cw=cw, d=d, xs=xs: e.matmul(
                            ps[:, 0:cw], W1[:, d, g, r, :], xs[g][:, c0:c0 + cw], start=(g == 0), stop=(g == 1)),
                            reads=[W1.d, xs[g].d], writes=[ps.d])
                    dst = Sre if r == 0 else Sim
                    pb.op("act", lambda e, ps=ps, dst=dst, o0=o0, cw=cw: e.activation(out=dst[:, o0:o0 + cw], in_=ps[:, 0:cw], func=AF.Copy),
                          reads=[ps.d], writes=[dst.d])
            if stop <= 7:
                return pb.finish()
            B1 = (b1, b1.t[:]); B2 = (b2, b2.t[:]); B3 = (b3, b3.t[:]); BI = (bI, bI.t[:])
            CO = (cosT, cosT.t[:]); SI = (sinT, sinT.t[:])
            ew.ts(B1, (ktab, ktab.t[:]), c(8), ALU.mult)
            ew.frac(B2, B1, BI, B3)
            ew.act(SI, B2, AF.Sin, scale=SIN_SCALE)
            ew.ts(B1, B1, 0.25, ALU.add)
            ew.frac(B2, B1, BI, B3)
            ew.act(CO, B2, AF.Sin, scale=SIN_SCALE)
            SR = (Sre, Sre.t[:]); SM = (Sim, Sim.t[:])
            sgn = 1.0 if d == 0 else -1.0
            ew.tt(B1, CO, SR, ALU.mult); ew.tt(B2, SI, SM, ALU.mult)
            ew.tt(B1, B1, B2, ALU.add if d == 0 else ALU.subtract)
            ew.tt(B2, CO, SM, ALU.mult); ew.tt(B3, SI, SR, ALU.mult)
            ew.tt(B2, B2, B3, ALU.subtract if d == 0 else ALU.add)
            if stop <= 8:
                return pb.finish()
            rho = col.t[:, 7:8].to_broadcast([128, NK])
            if d == 0:
                pb.op("dve", lambda e: e.tensor_tensor_scan(out=Sre[:], data0=rho, data1=b1[:], initial=0.0, op0=ALU.mult, op1=ALU.add),
                      reads=[col.d, b1.d], writes=[Sre.d])
                pb.op("dve", lambda e: e.tensor_tensor_scan(out=Sim[:], data0=rho, data1=b2[:], initial=0.0, op0=ALU.mult, op1=ALU.add),
                      reads=[col.d, b2.d], writes=[Sim.d])
            else:
                pb.op("dve", lambda e: e.tensor_tensor_scan(out=Sre[:, ::-1], data0=rho, data1=b1[:, ::-1], initial=0.0, op0=ALU.mult, op1=ALU.add),
                      reads=[col.d, b1.d], writes=[Sre.d])
                pb.op("dve", lambda e: e.tensor_tensor_scan(out=Sim[:, ::-1], data0=rho, data1=b2[:, ::-1], initial=0.0, op0=ALU.mult, op1=ALU.add),
                      reads=[col.d, b2.d], writes=[Sim.d])
            if stop <= 9:
                return pb.finish()
            hoff = 2 if d == 0 else 0
            HR = (Hb[d][0], Hb[d][0].t[:, hoff:hoff + NK]); HI = (Hb[d][1], Hb[d][1].t[:, hoff:hoff + NK])
            ew.tt(B1, CO, SR, ALU.mult); ew.tt(B2, SI, SM, ALU.mult)
            ew.tt(HR, B1, B2, ALU.subtract if d == 0 else ALU.add)
            ew.tt(B1, CO, SM, ALU.mult); ew.tt(B2, SI, SR, ALU.mult)
            ew.tt(HI, B1, B2, ALU.add if d == 0 else ALU.subtract)
        if stop <= 10:
            return pb.finish()
        for g in range(2):
            pb.dma("sp", pdd[:, g], pj_d[2 * p + g], [pj_d.d], [pdd.d])
            a0 = (tA[0], tA[0].t[:]); a1 = (tA[1], tA[1].t[:])
            ew.tt(a0, (ps_w, ps_w.t[:, (2 * g) * 128:(2 * g) * 128 + 128]), (cst, cst.t[:, 4, :]), ALU.mult, "dve")
            ew.tt(a1, (ps_w, ps_w.t[:, (2 * g + 1) * 128:(2 * g + 1) * 128 + 128]), (cst, cst.t[:, 5, :]), ALU.mult, "dve")
            ew.tt(a0, a0, a1, ALU.add, "dve")
            ew.ts(a1, (cst, cst.t[:, 6, :]), (pdd, pdd.t[:, g, :]), ALU.mult)
            ew.tt((Wsum, Wsum.t[:, g, :]), a0, a1, ALU.add, "dve")
        if stop <= 11:
            return pb.finish()
        for g in range(2):
            yb = Yb[g]
            hs = slice(64 * g, 64 * g + 64)
            for ti, (c0, cw) in enumerate(tiles_nat):
                ps = ps_y[ti % 2]
                c1 = d1col(c0)
                mm = [(Wsum[:, g, :], xs[g][:, c0:c0 + cw], [Wsum.d, xs[g].d]),
                      (Wintz[:, g, 0, 0, :], Hb[0][0][:, c0 + 1:c0 + 1 + cw], [Wintz.d, Hb[0][0].d]),
                      (Wintz[:, g, 0, 1, :], Hb[0][1][:, c0 + 1:c0 + 1 + cw], [Wintz.d, Hb[0][1].d]),
                      (Wintz[:, g, 1, 0, :], Hb[1][0][:, c1 + 1:c1 + 1 + cw], [Wintz.d, Hb[1][0].d]),
                      (Wintz[:, g, 1, 1, :], Hb[1][1][:, c1 + 1:c1 + 1 + cw], [Wintz.d, Hb[1][1].d])]
                for mi, (l, r_, rd) in enumerate(mm):
                    pb.op("pe", lambda e, ps=ps, l=l, r_=r_, mi=mi, cw=cw: e.matmul(ps[:, 0:cw], l, r_, start=(mi == 0), stop=(mi == 4)),
                          reads=rd, writes=[ps.d])
                pb.op("act", lambda e, ps=ps, yb=yb, c0=c0, cw=cw: e.activation(out=yb[:, c0:c0 + cw], in_=ps[:, 0:cw], func=AF.Gelu_apprx_tanh),
                      reads=[ps.d], writes=[yb.d])
            pb.dma("act", Yg[2 * p + g], yb[:], [yb.d], [Yg.dk(2 * p + g)])
    return pb.finish()


def build_progC(D, F, T, NCTX, NH, TW):
    pb = PB()
    KC = D // 128
    FC = F // 128
    Tres = T // NH
    tiles = [(i * TW, TW) for i in range(Tres // TW)]
    xT = pb.din("xT", [D, T], F32)
    gT = pb.din("gT", [D, T], BF16)
    w_glu = pb.din("w_glu", [D, 2 * D], F32)
    w_gu = pb.din("w_gu", [D, 2 * F], F32)
    w_down = pb.din("w_down", [F, D], F32)
    modP = pb.din("modP", [128, 6, KC, 2], F32)
    ngP = pb.din("ngP", [128, 4, KC], F32)
    xo = pb.dout("xT_out", [D, T], F32)
    zT = pb.dscr("zT", [D, T], F32)
    x1T = pb.dscr("x1T", [D, T], F32)
    uT = pb.dscr("uT", [F, T], BF16)
    yT = pb.dscr("yT", [D, T], F32)

    tabs = make_tables(pb, modP, ngP, KC)
    cm = Common(pb, T, TW)
    KM = max(KC, FC)
    xres = pb.sb("xres", [128, KM, Tres], BF16)
    ws = WStream(pb, KM)
    psums = [pb.ps("psl%d" % i) for i in range(4)]
    rstd = pb.sb("rstd", [128, T], F32)
    blk = [pb.sb("blk%d" % i, [128, Tres], F32) for i in range(2)]
    blkb = [pb.sb("blkb%d" % i, [128, Tres], BF16) for i in range(2)]
    sg = [pb.sb("sg%d" % i, [128, TW], F32) for i in range(2)]
    cnt = [0]

    def load_res(src, KCn, t0):
        for kc in range(KCn):
            pb.dma("sp", xres[:, kc, :], src[kc * 128:(kc + 1) * 128, t0:t0 + Tres], [src.dk(kc)], [xres.d])

    def ssq_acc(b, t0, first):
        if first:
            pb.op("act", lambda e: e.activation(out=cm.ssq[:, t0:t0 + Tres], in_=b[:], func=AF.Square),
                  reads=[b.d], writes=[cm.ssq.d])
        else:
            tm = cm.tmp[cnt[0] % 2]
            cnt[0] += 1
            pb.op("act", lambda e: e.activation(out=tm[:, 0:Tres], in_=b[:], func=AF.Square), reads=[b.d], writes=[tm.d])
            pb.op("dve", lambda e: e.tensor_tensor(out=cm.ssq[:, t0:t0 + Tres], in0=cm.ssq[:, t0:t0 + Tres],
                                                   in1=tm[:, 0:Tres], op=ALU.add),
                  reads=[tm.d, cm.ssq.d], writes=[cm.ssq.d])

    def gated_epi(func, dst, t0, out_bf16):
        def epi(gi, grp, ti, cc, pss):
            c0, cw = cc
            b = (blkb if out_bf16 else blk)[gi % 2]
            s = sg[(gi * len(tiles) + ti) % 2]
            g_ps, u_ps = (pss[0], pss[1]) if func == AF.Silu else (pss[1], pss[0])
            pb.op("act", lambda e: e.activation(out=s[:, 0:cw], in_=g_ps[:, 0:cw], func=func), reads=[g_ps.d], writes=[s.d])
            pb.op("dve", lambda e: e.tensor_tensor(out=b[:, c0:c0 + cw], in0=u_ps[:, 0:cw], in1=s[:, 0:cw], op=ALU.mult),
                  reads=[u_ps.d, s.d], writes=[b.d])
            if ti == len(tiles) - 1:
                j = grp[0]
                pb.dma("sp", dst[j * 128:(j + 1) * 128, t0:t0 + Tres], b[:], [b.d], [dst.dk(j)])
                if not out_bf16:
                    ssq_acc(b, t0, gi == 0)
        return epi

    for h in range(NH):
        t0 = h * Tres
        load_res(gT, KC, t0)
        linear(pb, ws, xres, KC, lambda nb: w_glu[:, nb * 128:(nb + 1) * 128], w_glu.d,
               [(j, KC + j) for j in range(KC)], tiles, psums, gated_epi(AF.Sigmoid, zT, t0, False))
    finalize_stats(pb, cm, D, rstd)
    residual_pass(pb, cm, zT, xT, x1T, rstd, tabs, "m", KC, NCTX, accumulate_ssq=True)
    finalize_stats(pb, cm, D, rstd)
    for h in range(NH):
        t0 = h * Tres
        modulate_pass(pb, cm, x1T, rstd, tabs, "f", xres, t0, Tres, KC, NCTX)
        linear(pb, ws, xres, KC, lambda nb: w_gu[:, nb * 128:(nb + 1) * 128], w_gu.d,
               [(j, FC + j) for j in range(FC)], tiles, psums, gated_epi(AF.Silu, uT, t0, True))
    for h in range(NH):
        t0 = h * Tres
        load_res(uT, FC, t0)

        def epi(gi, grp, ti, cc, pss, t0=t0):
            c0, cw = cc
            b = blk[gi % 2]
            pb.op("act", lambda e: e.activation(out=b[:, c0:c0 + cw], in_=pss[0][:, 0:cw], func=AF.Copy),
                  reads=[pss[0].d], writes=[b.d])
            if ti == len(tiles) - 1:
                j = grp[0]
                pb.dma("sp", yT[j * 128:(j + 1) * 128, t0:t0 + Tres], b[:], [b.d], [yT.dk(j)])
                ssq_acc(b, t0, gi == 0)
        linear(pb, ws, xres, FC, lambda nb: w_down[:, nb * 128:(nb + 1) * 128], w_down.d,
               [(j,) for j in range(KC)], tiles, psums, epi, prefetch=2)
    finalize_stats(pb, cm, D, rstd)
    residual_pass(pb, cm, yT, x1T, xo, rstd, tabs, "f", KC, NCTX)
    return pb.finish()


def build_progD(D, T, NCTX, NH, TW, TB, scale):
    pb = PB()
    KC = D // 128
    Tres = T // NH
    tiles = [(i * TW, TW) for i in range(Tres // TW)]
    NTB = Tres // TB
    xT = pb.din("xT", [D, T], F32)
    modP = pb.din("modP", [128, 6, KC, 2], F32)
    ngP = pb.din("ngP", [128, 4, KC], F32)
    w_qkv = pb.din("w_qkv", [D, 3 * D], F32)
    qT = pb.dout("qT", [D, T], BF16)
    kT = pb.dout("kT", [D, T], BF16)
    V = pb.dout("V", [T, D], BF16)
    tabs = make_tables(pb, modP, ngP, KC)
    cm = Common(pb, T, TW)
    rstd = pb.sb("rstd", [128, T], F32)
    xres = pb.sb("xres", [128, KC, Tres], BF16)
    ws = WStream(pb, KC)
    psums = [pb.ps("psl%d" % i) for i in range(2)]
    psv = [pb.ps("psv%d" % i) for i in range(2)]
    blkb = [pb.sb("blkb%d" % i, [128, Tres], BF16) for i in range(2)]
    vblk = [pb.sb("vblk%d" % i, [128, NTB, 128], BF16) for i in range(2)]
    stats_pass(pb, cm, xT, KC, D, rstd)
    for h in range(NH):
        t0 = h * Tres
        modulate_pass(pb, cm, xT, rstd, tabs, "m", xres, t0, Tres, KC, NCTX)

        def epi(gi, grp, ti, cc, pss, t0=t0):
            c0, cw = cc
            j = grp[0]
            b = blkb[gi % 2]
            sc = scale if j < KC else 1.0
            pb.op("act", lambda e: e.activation(out=b[:, c0:c0 + cw], in_=pss[0][:, 0:cw], func=AF.Copy, scale=sc),
                  reads=[pss[0].d], writes=[b.d])
            if ti == len(tiles) - 1:
                dst = qT if j < KC else kT
                jj = j % KC
                pb.dma("act", dst[jj * 128:(jj + 1) * 128, t0:t0 + Tres], b[:], [b.d], [dst.dk(jj)])
        linear(pb, ws, xres, KC, lambda nb: w_qkv[:, nb * 128:(nb + 1) * 128], w_qkv.d,
               [(j,) for j in range(2 * KC)], tiles, psums, epi, prefetch=2)
        slabs = {}
        for j in range(min(2, KC)):
            slabs[j] = ws.fetch(w_qkv[:, (2 * KC + j) * 128:(2 * KC + j + 1) * 128], KC, w_qkv.d)
        cnt = 0
        for j in range(KC):
            if j + 2 < KC:
                slabs[j + 2] = ws.fetch(w_qkv[:, (2 * KC + j + 2) * 128:(2 * KC + j + 3) * 128], KC, w_qkv.d)
            sl = slabs.pop(j)
            vb = vblk[j % 2]
            for tb in range(NTB):
                ps = psv[cnt % 2]
                cnt += 1
                for kc in range(KC):
                    pb.op("pe", lambda e, ps=ps, sl=sl, kc=kc, tb=tb: e.matmul(
                        ps[0:TB, 0:128], xres[:, kc, tb * TB:(tb + 1) * TB], sl[:, kc, :], start=(kc == 0), stop=(kc == KC - 1)),
                        reads=[sl.d, xres.d], writes=[ps.d])
                pb.op("act", lambda e, ps=ps, vb=vb, tb=tb: e.activation(out=vb[0:TB, tb, :], in_=ps[0:TB, 0:128], func=AF.Copy),
                      reads=[ps.d], writes=[vb.d])
            pb.dma("act", V[t0:t0 + Tres, j * 128:(j + 1) * 128].rearrange("(tb p) n -> p tb n", p=TB), vb[0:TB, :, :],
                   [vb.d], [V.dk((h, j))])
    return pb.finish()


def build_progE1(NHEAD, T, NCTX, NCK_ALL, NQT, NEXT):
    pb = PB()
    D = NHEAD * 128
    NXK = NEXT * 128
    NCC = NCK_ALL // 128
    qT = pb.din("qT", [D, T], BF16)
    kTx = pb.din("kTx", [D, NXK], BF16)
    kTc = pb.din("kTc", [D, NCK_ALL], BF16)
    Vx = pb.din("Vx", [NXK, D], BF16)
    Vc = pb.din("Vc", [NCK_ALL, D], BF16)
    btab = pb.din("btab", [NHEAD, 128, 5, 768], BF16)
    identd = pb.din("ident", [128, 128], BF16)
    out = pb.dout("attnT", [D, T], BF16)
    ident = pb.sb("ident_s", [128, 128], BF16)
    onesb = pb.sb("onesb", [128, 128], BF16)
    pb.dma("sp", ident[:], identd[:], [identd.d], [ident.d])
    pb.op("dve", lambda e: e.memset(onesb[:], 1.0), writes=[onesb.d])
    NK = NXK + NCK_ALL
    kh = [pb.sb("kh%d" % i, [128, NK], BF16) for i in range(2)]
    vh = [pb.sb("vh%d" % i, [128, NEXT + NCC, 128], BF16) for i in range(2)]
    qh = [pb.sb("qh%d" % i, [128, T], BF16) for i in range(2)]
    bt = [pb.sb("bt%d" % i, [128, 5, 768], BF16) for i in range(2)]
    ob = [pb.sb("ob%d" % i, [128, T], BF16) for i in range(2)]
    ET = [pb.sb("ET%d" % i, [128, 8, 128], BF16) for i in range(3)]
    rden = [pb.sb("rden%d" % i, [128, 128], F32) for i in range(2)]
    ps_s = [pb.ps("ps_s%d" % i) for i in range(4)]
    ps_o = [pb.ps("ps_o%d" % i) for i in range(2)]

    def load_head(h):
        i = h % 2
        rows = slice(h * 128, (h + 1) * 128)
        pb.dma("sp", kh[i][:, 0:NXK], kTx[rows, :], [kTx.d], [kh[i].d])
        pb.dma("sp", kh[i][:, NXK:NK], kTc[rows, :], [kTc.d], [kh[i].d])
        pb.dma("sp", vh[i][:, 0:NEXT, :], Vx[:, rows].rearrange("(c p) d -> p c d", p=128), [Vx.d], [vh[i].d])
        pb.dma("sp", vh[i][:, NEXT:NEXT + NCC, :], Vc[:, rows].rearrange("(c p) d -> p c d", p=128), [Vc.d], [vh[i].d])
        pb.dma("sp", qh[i][:], qT[rows, :], [qT.d], [qh[i].d])
        pb.dma("sp", bt[i][:], btab[h], [btab.d], [bt[i].d])
    qtiles = []
    ctx_chunks = [(NEXT + c, None) for c in range(NCC)]
    qtiles.append((0, NCTX, list(ctx_chunks)))
    for i in range(NQT):
        if i == 0:
            cl, lo, n = 0, 0, 6
        elif i == NQT - 1:
            cl, lo, n = 4, NEXT - 6, 6
        else:
            cl = 1 if i == 1 else (3 if i == NQT - 2 else 2)
            lo, n = i, 5
        qtiles.append((NCTX + i * 128, 128, [(lo + m, (cl, m)) for m in range(n)] + ctx_chunks))
    load_head(0)
    cnt = 0
    for h in range(NHEAD):
        if h + 1 < NHEAD:
            load_head(h + 1)
        i = h % 2
        o = ob[i]
        for (q0, qw, chunks) in qtiles:
            et = ET[cnt % 3]
            po = ps_o[cnt % 2]
            pss = [ps_s[(cnt % 2) * 2], ps_s[(cnt % 2) * 2 + 1]]
            cnt += 1
            nch = len(chunks)
            for ci, (kc, tb) in enumerate(chunks):
                ps = pss[ci // 4]
                col = (ci % 4) * 128
                pb.op("pe", lambda e, ps=ps, col=col, kc=kc, i=i, q0=q0, qw=qw, tb=tb: e.matmul(
                    ps[:, col:col + qw], kh[i][:, kc * 128:(kc + 1) * 128], qh[i][:, q0:q0 + qw], start=True, stop=(tb is None)),
                    reads=[kh[i].d, qh[i].d], writes=[ps.d])
                if tb is not None:
                    pb.op("pe", lambda e, ps=ps, col=col, i=i, qw=qw, tb=tb: e.matmul(
                        ps[:, col:col + qw], bt[i][:, tb[0], tb[1] * 128:(tb[1] + 1) * 128], ident[:, 0:qw], start=False, stop=True),
                        reads=[bt[i].d, ident.d], writes=[ps.d])
            for bi in range((nch + 3) // 4):
                nb = min(4, nch - 4 * bi)
                pb.op("act", lambda e, ps=pss[bi], et=et, bi=bi, nb=nb, qw=qw: e.activation(
                    out=et[:, 4 * bi:4 * bi + nb, 0:qw], in_=ps[:, 0:nb * 128].rearrange("p (c q) -> p c q", q=128)[:, :, 0:qw], func=AF.Exp),
                    reads=[pss[bi].d], writes=[et.d])
            for ci, (kc, tb) in enumerate(chunks):
                pb.op("pe", lambda e, po=po, ci=ci, kc=kc, i=i, et=et, qw=qw, nch=nch: e.matmul(
                    po[:, 0:qw], vh[i][:, kc, :], et[:, ci, 0:qw], start=(ci == 0), stop=(ci == nch - 1)),
                    reads=[vh[i].d, et.d], writes=[po.d])
            for ci, (kc, tb) in enumerate(chunks):
                pb.op("pe", lambda e, po=po, ci=ci, et=et, qw=qw, nch=nch: e.matmul(
                    po[:, 128:128 + qw], onesb[:], et[:, ci, 0:qw], start=(ci == 0), stop=(ci == nch - 1)),
                    reads=[onesb.d, et.d], writes=[po.d])
            rd = rden[cnt % 2]
            pb.op("dve", lambda e, po=po, rd=rd, qw=qw: e.reciprocal(out=rd[:, 0:qw], in_=po[:, 128:128 + qw]), reads=[po.d], writes=[rd.d])
            pb.op("dve", lambda e, po=po, rd=rd, o=o, q0=q0, qw=qw: e.tensor_tensor(
                out=o[:, q0:q0 + qw], in0=po[:, 0:qw], in1=rd[:, 0:qw], op=ALU.mult), reads=[po.d, rd.d], writes=[o.d])
        pb.dma("act", out[h * 128:(h + 1) * 128, :], o[:], [o.d], [out.dk(h)])
    return pb.finish()


def build_progE2(D, E, NE, T, NCTX, TW, TB):
    pb = PB()
    KC = D // 128
    EC = E // 128
    NH, NQ = 2, 4
    Tres = T // NH
    Tq = T // NQ
    tiles = [(i * TW, TW) for i in range(Tres // TW)]
    tiles_q = [(i * TW, TW) for i in range(Tq // TW)]
    NTB = Tres // TB
    KD = NE * EC
    KS = KD // KC
    xT = pb.din("xT", [D, T], F32)
    attnT = pb.din("attnT", [D, T], BF16)
    w_o = pb.din("w_o", [D, D], F32)
    modP = pb.din("modP", [128, 6, KC, 2], F32)
    ngP = pb.din("ngP", [128, 4, KC], F32)
    wrP = pb.din("wrP", [128, KC, NE], F32)
    w_gu = pb.din("moe_w_gu", [NE, D, 2 * E], F32)
    w_dn = pb.din("moe_w_down", [NE * E, D], F32)
    identd = pb.din("identf", [128, 128], F32)
    xo = pb.dout("xT_out", [D, T], F32)
    yT = pb.dscr("yT", [D, T], F32)
    x1T = pb.dscr("x1T", [D, T], F32)
    hT = pb.dscr("hT_all", [NE * E, T], BF16)

    tabs = make_tables(pb, modP, ngP, KC)
    cm = Common(pb, T, TW)
    xflat = pb.sb("xres", [128, KC * Tres], BF16)
    xres = TT(xflat.t[:].rearrange("p (k t) -> p k t", t=Tres), "xres_v"); xres.d = xflat.d
    xresq = TT(xflat.t[:].rearrange("p (k t) -> p k t", t=Tq), "xres_q"); xresq.d = xflat.d
    ws = WStream(pb, KC)
    psums = [pb.ps("psl%d" % i) for i in range(4)]
    ps_r = [pb.ps("psr%d" % i) for i in range(2)]
    rstd = pb.sb("rstd", [128, T], F32)
    blk = [pb.sb("blk%d" % i, [128, Tres], F32) for i in range(2)]
    blkb = [pb.sb("blkb%d" % i, [128, Tres], BF16) for i in range(2)]
    sg = [pb.sb("sg%d" % i, [128, TW], F32) for i in range(2)]
    sg2 = sg
    identf = pb.sb("identf_s", [128, 128], F32)
    pb.dma("act", identf[:], identd[:], [identd.d], [identf.d])
    wrf = pb.sb("wrf", [128, KC, NE], F32)
    wrb = pb.sb("wrb", [128, KC, NE], BF16)
    pb.dma("act", wrf[:], wrP[:], [wrP.d], [wrf.d])
    pb.op("dve", lambda e: e.tensor_copy(out=wrb[:], in_=wrf[:]), reads=[wrf.d], writes=[wrb.d])
    gall = pb.sb("gall", [128, NTB, NE], F32)
    gb = [pb.sb("gb0", [128, Tres], F32)] * 2
    grep = [pb.sb("grep%d" % i, [128, 128], F32) for i in range(2)]
    rt = [pb.sb("rt%d" % i, [128, NE], F32) for i in range(6)]
    cnt = [0]

    def ssq_acc(b, t0, w, first):
        if first:
            pb.op("act", lambda e: e.activation(out=cm.ssq[:, t0:t0 + w], in_=b[:, 0:w], func=AF.Square),
                  reads=[b.d], writes=[cm.ssq.d])
        else:
            tm = cm.tmp[cnt[0] % 2]
            cnt[0] += 1
            pb.op("act", lambda e: e.activation(out=tm[:, 0:w], in_=b[:, 0:w], func=AF.Square), reads=[b.d], writes=[tm.d])
            pb.op("dve", lambda e: e.tensor_tensor(out=cm.ssq[:, t0:t0 + w], in0=cm.ssq[:, t0:t0 + w], in1=tm[:, 0:w], op=ALU.add),
                  reads=[tm.d, cm.ssq.d], writes=[cm.ssq.d])

    def plain_epi(t0, w, ntiles):
        def epi(gi, grp, ti, cc, pss):
            c0, cw = cc
            b = blk[gi % 2]
            pb.op("act", lambda e: e.activation(out=b[:, c0:c0 + cw], in_=pss[0][:, 0:cw], func=AF.Copy),
                  reads=[pss[0].d], writes=[b.d])
            if ti == ntiles - 1:
                j = grp[0]
                pb.dma("sp", yT[j * 128:(j + 1) * 128, t0:t0 + w], b[:, 0:w], [b.d], [yT.dk(j)])
                ssq_acc(b, t0, w, gi == 0)
        return epi

    for h in range(NH):
        t0 = h * Tres
        for kc in range(KC):
            pb.dma("sp", xres[:, kc, :], attnT[kc * 128:(kc + 1) * 128, t0:t0 + Tres], [attnT.dk(kc)], [xres.d])
        linear(pb, ws, xres, KC, lambda nb: w_o[:, nb * 128:(nb + 1) * 128], w_o.d,
               [(j,) for j in range(KC)], tiles, psums, plain_epi(t0, Tres, len(tiles)), prefetch=2)
    finalize_stats(pb, cm, D, rstd)
    residual_pass(pb, cm, yT, xT, x1T, rstd, tabs, "m", KC, NCTX, accumulate_ssq=True)
    finalize_stats(pb, cm, D, rstd)
    for h in range(NH):
        t0 = h * Tres
        modulate_pass(pb, cm, x1T, rstd, tabs, "f", xres, t0, Tres, KC, NCTX)
        for tb in range(NTB):
            ps = ps_r[tb % 2]
            for kc in range(KC):
                pb.op("pe", lambda e, ps=ps, kc=kc, tb=tb: e.matmul(ps[0:TB, 0:NE], xres[:, kc, tb * TB:(tb + 1) * TB], wrb[:, kc, :],
                                                                    start=(kc == 0), stop=(kc == KC - 1)),
                      reads=[xres.d, wrb.d], writes=[ps.d])
            lg, mx, nm, ex, mk, e2 = rt
            P = slice(0, TB)
            pb.op("act", lambda e, ps=ps: e.activation(out=lg[P, :], in_=ps[P, 0:NE], func=AF.Copy), reads=[ps.d], writes=[lg.d])
            pb.op("dve", lambda e: e.max(out=mx[P, 0:8], in_=lg[P, :]), reads=[lg.d], writes=[mx.d])
            pb.op("dve", lambda e: e.tensor_scalar(out=nm[P, 0:1], in0=mx[P, 0:1], scalar1=-1.0, scalar2=None, op0=ALU.mult),
                  reads=[mx.d], writes=[nm.d])
            pb.op("act", lambda e: e.activation(out=ex[P, :], in_=lg[P, :], func=AF.Exp, bias=nm[P, 0:1]), reads=[lg.d, nm.d], writes=[ex.d])
            pb.op("dve", lambda e: e.tensor_scalar(out=mk[P, :], in0=lg[P, :], scalar1=mx[P, 1:2], scalar2=None, op0=ALU.is_ge),
                  reads=[lg.d, mx.d], writes=[mk.d])
            pb.op("dve", lambda e: e.tensor_tensor(out=ex[P, :], in0=ex[P, :], in1=mk[P, :], op=ALU.mult), reads=[ex.d, mk.d], writes=[ex.d])
            pb.op("act", lambda e: e.activation(out=e2[P, 0:1], in_=mx[P, 1:2], func=AF.Exp, bias=nm[P, 0:1]), reads=[mx.d, nm.d], writes=[e2.d])
            pb.op("dve", lambda e: e.tensor_scalar(out=e2[P, 0:1], in0=e2[P, 0:1], scalar1=1.0, scalar2=None, op0=ALU.add),
                  reads=[e2.d], writes=[e2.d])
            pb.op("dve", lambda e: e.reciprocal(out=e2[P, 0:1], in_=e2[P, 0:1]), reads=[e2.d], writes=[e2.d])
            pb.op("dve", lambda e, tb=tb: e.tensor_scalar(out=gall[P, tb, :], in0=ex[P, :], scalar1=e2[P, 0:1], scalar2=None, op0=ALU.mult),
                  reads=[ex.d, e2.d], writes=[gall.d])
        for ex_i in range(NE):
            g = gb[ex_i % 2]
            for tb in range(NTB):
                gr = grep[tb % 2]
                ps = ps_r[tb % 2]
                P = slice(0, TB)
                pb.op("dve", lambda e, gr=gr, tb=tb, ex_i=ex_i: e.tensor_copy(out=gr[P, :], in_=gall[P, tb, ex_i:ex_i + 1].to_broadcast([TB, 128])),
                      reads=[gall.d], writes=[gr.d])
                pb.op("pe", lambda e, ps=ps, gr=gr: e.matmul(ps[:, 0:TB], gr[P, :], identf[P, 0:TB], start=True, stop=True),
                      reads=[gr.d, identf.d], writes=[ps.d])
                pb.op("act", lambda e, ps=ps, g=g, tb=tb: e.activation(out=g[:, tb * TB:(tb + 1) * TB], in_=ps[:, 0:TB], func=AF.Copy),
                      reads=[ps.d], writes=[g.d])

            def epi(gi, grp, ti, cc, pss, g=g, ex_i=ex_i, t0=t0):
                c0, cw = cc
                b = blkb[gi % 2]
                s = sg[(gi * len(tiles) + ti) % 2]
                s2 = sg2[(gi * len(tiles) + ti) % 2]
                pb.op("act", lambda e: e.activation(out=s[:, 0:cw], in_=pss[0][:, 0:cw], func=AF.Silu), reads=[pss[0].d], writes=[s.d])
                pb.op("dve", lambda e: e.tensor_tensor(out=s2[:, 0:cw], in0=pss[1][:, 0:cw], in1=s[:, 0:cw], op=ALU.mult),
                      reads=[pss[1].d, s.d], writes=[s2.d])
                pb.op("pool", lambda e: e.tensor_tensor(out=b[:, c0:c0 + cw], in0=s2[:, 0:cw], in1=g[:, c0:c0 + cw], op=ALU.mult),
                      reads=[s2.d, g.d], writes=[b.d])
                if ti == len(tiles) - 1:
                    j = ex_i * EC + grp[0]
                    pb.dma("sp", hT[j * 128:(j + 1) * 128, t0:t0 + Tres], b[:], [b.d], [hT.dk(j)])
            linear(pb, ws, xres, KC, lambda nb, ex_i=ex_i: w_gu[ex_i, :, nb * 128:(nb + 1) * 128], w_gu.d,
                   [(j, EC + j) for j in range(EC)], tiles, psums, epi)
    for qd in range(NQ):
        t0 = qd * Tq
        for kc in range(KD):
            pb.dma("sp", xresq[:, kc, :], hT[kc * 128:(kc + 1) * 128, t0:t0 + Tq], [hT.dk(kc)], [xresq.d])
        linear(pb, ws, xresq, KC, lambda nb, s_: w_dn[s_ * KC * 128:(s_ + 1) * KC * 128, nb * 128:(nb + 1) * 128], w_dn.d,
               [(j,) for j in range(KC)], tiles_q, psums, plain_epi(t0, Tq, len(tiles_q)), prefetch=1, KS=KS)
    finalize_stats(pb, cm, D, rstd)
    residual_pass(pb, cm, yT, x1T, xo, rstd, tabs, "f", KC, NCTX)
    return pb.finish()


D_MODEL = 4096
SEQ = 16384
CTXL = 256
NCORE = 8
TX = SEQ // NCORE
NCTX = CTXL // NCORE
T = TX + NCTX
KC = D_MODEL // 128
DEPTH = 4
_PROGS = {}
N_LAUNCH = [0]


def _prog(name, fn):
    if name not in _PROGS:
        _PROGS[name] = fn()
    return _PROGS[name]


def _run(nc, in_maps):
    N_LAUNCH[0] += 1
    res = run_bass_kernel_spmd(nc, in_maps, core_ids=list(range(NCORE)))
    return res.results


def kernel(x, c, ctx, c_ctx, ada_w, ada_b, norm_g,
           s5_a_re, s5_a_im, s5_log_dt, s5_b_re, s5_b_im, s5_c_re, s5_c_im, s5_d, s5_w_glu,
           na_w_qkv, na_w_o, na_rpb, ffn_w_gu, ffn_w_down, moe_w_router, moe_w_gu, moe_w_down):
    f32 = np.float32
    A = lambda a: np.ascontiguousarray(np.asarray(a))
    x = np.asarray(x, f32); ctx = np.asarray(ctx, f32)
    ada_w = np.asarray(ada_w); ada_b = np.asarray(ada_b); norm_g = np.asarray(norm_g)
    NB = DEPTH * 6 * KC // NCORE
    progP = _prog("P", lambda: build_progP(D_MODEL, NB))
    cond = np.stack([np.asarray(c, f32)[0], np.asarray(c_ctx, f32)], 0)
    condP = A(cond.reshape(2, KC, 128).transpose(2, 1, 0))
    ims = []
    per_layer = 6 * KC // NB
    for i in range(NCORE):
        l, hf = i // per_layer, i % per_layer
        cs = slice(hf * NB * 128, (hf + 1) * NB * 128)
        ims.append(dict(condP=condP, wP=A(ada_w[l][:, cs]), bP=A(ada_b[l][cs].reshape(NB, 128).T)))
    r = _run(progP, ims)
    modP = []
    for l in range(DEPTH):
        m = np.concatenate([r[l * per_layer + hf]["modT"] for hf in range(per_layer)], axis=1)
        modP.append(A(m.reshape(128, 6, KC, 2)))
    ngP = [A(norm_g[l].reshape(4, KC, 128).transpose(2, 0, 1)) for l in range(DEPTH)]
    xT = [A(np.concatenate([ctx[0, NCTX * i:NCTX * (i + 1)], x[0, TX * i:TX * (i + 1)]], 0).T) for i in range(NCORE)]
    identb = np.eye(128, dtype=f32).astype(NP_BF16)
    identf = np.eye(128, dtype=f32)
    for l in range(DEPTH):
        j = l // 2
        if l % 2 == 0:
            progA = _prog("A", lambda: build_progA(D_MODEL, T, NCTX))
            r = _run(progA, [dict(xT=xT[i], modP=modP[l], ngP=ngP[l]) for i in range(NCORE)])
            Xloc = [r[i]["Xloc"] for i in range(NCORE)]
            GPC = 256 // NCORE
            NKC, NKX = NCTX // 8, TX // 8
            consts, cpart, ktab = s5_consts(NCORE * (NKC + NKX))
            ims = []
            for d in range(NCORE):
                gs = slice(GPC * d, GPC * (d + 1))
                parts = [Xloc[s][gs, :, :, 0:NKC] for s in range(NCORE)] + [Xloc[s][gs, :, :, NKC:] for s in range(NCORE)]
                Xg = np.concatenate(parts, axis=3).reshape(GPC, 128, NCORE * (NKC + NKX))
                prm = s5_params(np.asarray(s5_a_re[j]), np.asarray(s5_a_im[j]), np.asarray(s5_log_dt[j]), np.asarray(s5_b_re[j]),
                                np.asarray(s5_b_im[j]), np.asarray(s5_c_re[j]), np.asarray(s5_c_im[j]), np.asarray(s5_d[j]),
                                list(range(GPC * d, GPC * (d + 1))))
                ims.append(dict(Xg=A(Xg), consts=consts, cpart=cpart, ktab=ktab, **prm))
            progB = _prog("B", lambda: build_progB(GPC // 2, NCORE * NKC, 4, NCORE * NKX // 4))
            r = _run(progB, ims)
            Yg = [r[d]["Yg"].reshape(GPC, 8, 16, NCORE * (NKC + NKX)) for d in range(NCORE)]
            ims = []
            for s in range(NCORE):
                cols = []
                for d in range(NCORE):
                    yc = Yg[d][:, :, :, NKC * s:NKC * (s + 1)]
                    yx = Yg[d][:, :, :, NCORE * NKC + NKX * s:NCORE * NKC + NKX * (s + 1)]
                    y = np.concatenate([yc, yx], axis=3)
                    cols.append(y.transpose(0, 2, 3, 1).reshape(GPC * 16, T))
                gT = np.concatenate(cols, axis=0)
                ims.append(dict(xT=xT[s], gT=A(gT), w_glu=np.asarray(s5_w_glu[j]), w_gu=np.asarray(ffn_w_gu[j]),
                                w_down=np.asarray(ffn_w_down[j]), modP=modP[l], ngP=ngP[l]))
            progC = _prog("C", lambda: build_progC(D_MODEL, 4096, T, NCTX, 2, 260))
            r = _run(progC, ims)
            xT = [r[i]["xT_out"] for i in range(NCORE)]
        else:
            progD = _prog("D", lambda: build_progD(D_MODEL, T, NCTX, 2, 260, 104, 128 ** -0.5))
            r = _run(progD, [dict(xT=xT[i], modP=modP[l], ngP=ngP[l], w_qkv=np.asarray(na_w_qkv[j])) for i in range(NCORE)])
            qT = [r[i]["qT"] for i in range(NCORE)]
            kT = [r[i]["kT"] for i in range(NCORE)]
            V = [r[i]["V"] for i in range(NCORE)]
            kTc = A(np.concatenate([kT[i][:, 0:NCTX] for i in range(NCORE)], axis=1))
            Vc = A(np.concatenate([V[i][0:NCTX] for i in range(NCORE)], axis=0))
            HALO = 256
            zk = np.zeros((D_MODEL, HALO), NP_BF16)
            zv = np.zeros((HALO, D_MODEL), NP_BF16)
            rpb = np.asarray(na_rpb[j], f32)
            ims = []
            for i in range(NCORE):
                kb = kT[i - 1][:, T - HALO:] if i > 0 else zk
                ka = kT[i + 1][:, NCTX:NCTX + HALO] if i < NCORE - 1 else zk
                vb = V[i - 1][T - HALO:] if i > 0 else zv
                va = V[i + 1][NCTX:NCTX + HALO] if i < NCORE - 1 else zv
                btab = na_bias_tables(rpb, i, 32, 256, 16, 20).astype(NP_BF16)
                ims.append(dict(qT=qT[i], kTx=A(np.concatenate([kb, kT[i][:, NCTX:], ka], axis=1)), kTc=kTc,
                                Vx=A(np.concatenate([vb, V[i][NCTX:], va], axis=0)), Vc=Vc, btab=btab, ident=identb))
            progE1 = _prog("E1", lambda: build_progE1(32, T, NCTX, CTXL, 16, 20))
            r = _run(progE1, ims)
            attnT = [r[i]["attnT"] for i in range(NCORE)]
            wrP = A(np.asarray(moe_w_router[j]).reshape(KC, 128, 8).transpose(1, 0, 2))
            progE2 = _prog("E2", lambda: build_progE2(D_MODEL, 1024, 8, T, NCTX, 260, 104))
            ims = [dict(xT=xT[i], attnT=attnT[i], w_o=np.asarray(na_w_o[j]), modP=modP[l], ngP=ngP[l], wrP=wrP,
                        moe_w_gu=np.asarray(moe_w_gu[j]), moe_w_down=np.asarray(moe_w_down[j]).reshape(8 * 1024, D_MODEL),
                        identf=identf) for i in range(NCORE)]
            r = _run(progE2, ims)
            xT = [r[i]["xT_out"] for i in range(NCORE)]
    out = np.concatenate([xT[i][:, NCTX:].T for i in range(NCORE)], axis=0)[None]
    return np.ascontiguousarray(out.astype(f32))
```
